# Optimizing a Trainium2 kernel written in Bass

```python
import jax, jax.numpy as jnp
from jax import lax
import numpy as np

D_MODEL = 1024
BATCH = 8
SEQ = 4096
DEPTH = 2

CTX_LEN = 256
GRID_W = 64

RNN_WIDTH = 1024
RNN_HEADS = 8
RNN_BLOCK = RNN_WIDTH // RNN_HEADS
CONV_WIDTH = 4
CONV_LEFT = 2
LRU_C = 8.0

FOURIER_WIDTH = 512
FOURIER_GROUPS = 4
FOURIER_GROUP_DIM = FOURIER_WIDTH // FOURIER_GROUPS

N_BRANCHES = 2
IN_PROJ_WIDTH = 2 * RNN_WIDTH + FOURIER_WIDTH + N_BRANCHES * D_MODEL

N_EXPERTS = 16
N_EXPERT_GROUPS = 4
EXPERTS_PER_GROUP = N_EXPERTS // N_EXPERT_GROUPS
TOP_K = 2
D_EXPERT = 1024

N_MOD = 6
RMS_EPS = 1e-6

kernel_name = "hybrid_rglru_fourier_grouped_moe_dit"


def rms_norm(x, g):
    xf = x.astype(jnp.float32)
    y = xf * lax.rsqrt(jnp.mean(xf * xf, axis=-1, keepdims=True) + RMS_EPS)
    return (y * g.astype(jnp.float32)).astype(x.dtype)


def modulate(x, shift, scale):
    return x * (1 + scale) + shift


def centred_dwconv(u, w, b):
    L = u.shape[-2]
    pad = [(0, 0)] * (u.ndim - 2) + [(CONV_LEFT, CONV_WIDTH - 1 - CONV_LEFT), (0, 0)]
    up = jnp.pad(u, pad)
    out = b
    for k in range(CONV_WIDTH):
        out = out + up[..., k:k + L, :] * w[k]
    return out


def conv_tokens(u, w, b, on_grid):
    if on_grid:
        n_b, n_tok, ch = u.shape
        rows = n_tok // GRID_W
        return centred_dwconv(u.reshape(n_b, rows, GRID_W, ch), w, b).reshape(n_b, n_tok, ch)
    return centred_dwconv(u, w, b)


def rglru_coeffs(u, w_a, b_a, w_x, b_x, lam):
    n_b, n_tok, _ = u.shape
    uh = u.reshape(n_b, n_tok, RNN_HEADS, RNN_BLOCK)
    r = jax.nn.sigmoid(jnp.einsum('blhi,hij->blhj', uh, w_a) + b_a).reshape(n_b, n_tok, RNN_WIDTH)
    i = jax.nn.sigmoid(jnp.einsum('blhi,hij->blhj', uh, w_x) + b_x).reshape(n_b, n_tok, RNN_WIDTH)
    log_a = -LRU_C * r.astype(jnp.float32) * jax.nn.softplus(-lam.astype(jnp.float32))
    a = jnp.exp(log_a)
    mult = jnp.sqrt(-jnp.expm1(2.0 * log_a))
    return a, mult * (i * u).astype(jnp.float32)


def linear_scan(a, b, h0):
    b = b.at[:, 0].add(a[:, 0] * h0)

    def combine(left, right):
        a_l, b_l = left
        a_r, b_r = right
        return a_l * a_r, a_r * b_l + b_r

    _, h = lax.associative_scan(combine, (a, b), axis=1)
    return h


def rglru_bidirectional(u, h0_f, h0_b, lru_f, lru_b):
    a_f, b_f = rglru_coeffs(u, *lru_f)
    h_f = linear_scan(a_f, b_f, h0_f)
    a_b, b_b = rglru_coeffs(jnp.flip(u, 1), *lru_b)
    h_b = jnp.flip(linear_scan(a_b, b_b, h0_b), 1)
    return h_f, h_b


def fourier_mix(u):
    n_b, n_tok, _ = u.shape
    ug = u.astype(jnp.float32).reshape(n_b, n_tok, FOURIER_GROUPS, FOURIER_GROUP_DIM)
    y = jnp.fft.fft2(ug, axes=(1, 3), norm="ortho").real
    return y.reshape(n_b, n_tok, FOURIER_WIDTH).astype(u.dtype)


def mixer(h, on_grid, h0_f, h0_b, w_in, conv_w, conv_b, lru_f, lru_b, w_proj_rnn, w_proj_fourier, w_out):
    proj = h @ w_in
    s1 = RNN_WIDTH
    s2 = 2 * RNN_WIDTH
    s3 = s2 + FOURIER_WIDTH
    s4 = s3 + D_MODEL
    u_rnn, u_gate, u_four, g_rnn, g_four = jnp.split(proj, [s1, s2, s3, s4], axis=-1)
    u = conv_tokens(u_rnn, conv_w, conv_b, on_grid)
    h_f, h_b = rglru_bidirectional(u, h0_f, h0_b, lru_f, lru_b)
    y_rnn = ((h_f + h_b).astype(h.dtype) * jax.nn.gelu(u_gate)) @ w_proj_rnn
    y_four = fourier_mix(u_four) @ w_proj_fourier
    merged = jax.nn.sigmoid(g_rnn) * y_rnn + jax.nn.sigmoid(g_four) * y_four
    return merged @ w_out, h_f[:, -1], h_b[:, 0]


def context_scan_states(h, w_in, conv_w, conv_b, lru_f, lru_b):
    u = conv_tokens(h @ w_in[:, :RNN_WIDTH], conv_w, conv_b, False)
    zeros = jnp.zeros((h.shape[0], RNN_WIDTH), jnp.float32)
    h_f, h_b = rglru_bidirectional(u, zeros, zeros, lru_f, lru_b)
    return h_f[:, -1], h_b[:, 0]


def grouped_moe(h, router_w, router_b, w1, w3, w2):
    n_b, n_tok, d = h.shape
    t = h.reshape(-1, d)
    n_t = t.shape[0]
    aff = jax.nn.sigmoid((t @ router_w).astype(jnp.float32))
    sel = (aff + router_b.astype(jnp.float32)).reshape(n_t, N_EXPERT_GROUPS, EXPERTS_PER_GROUP)
    group_score = lax.top_k(sel, TOP_K)[0].sum(-1)
    grp = jnp.argmax(group_score, axis=-1)
    in_group = jax.nn.one_hot(grp, N_EXPERT_GROUPS, dtype=jnp.bool_)[:, :, None]
    masked = jnp.where(in_group, sel, -jnp.inf).reshape(n_t, N_EXPERTS)
    _, expert_idx = lax.top_k(masked, TOP_K)
    w_sel = jnp.take_along_axis(aff, expert_idx, axis=1)
    w_sel = w_sel / jnp.sum(w_sel, axis=-1, keepdims=True)
    gates = jnp.sum(jax.nn.one_hot(expert_idx, N_EXPERTS, dtype=jnp.float32) * w_sel[..., None], axis=1)
    gates = gates.astype(t.dtype)
    y = jnp.zeros_like(t)
    for e in range(N_EXPERTS):
        he = jax.nn.silu(t @ w1[e]) * (t @ w3[e])
        y = y + gates[:, e:e + 1] * (he @ w2[e])
    return y.reshape(n_b, n_tok, d)


def setup_inputs(seed: int = 0) -> dict:
    key = jax.random.key(seed)
    ks = jax.random.split(key, 32)
    f32 = jnp.float32

    def nrm(k, shape, scale):
        return jax.random.normal(k, shape, f32) * scale

    u_decay = jax.random.uniform(ks[20], (DEPTH, 2, RNN_WIDTH), f32, 0.9, 0.999)
    s = u_decay ** (1.0 / LRU_C)
    lru_lambda = jnp.log(s) - jnp.log1p(-s)

    return {
        "x": nrm(ks[0], (BATCH, SEQ, D_MODEL), 1.0),
        "c": nrm(ks[1], (BATCH, D_MODEL), 1.0),
        "ctx": nrm(ks[2], (BATCH, CTX_LEN, D_MODEL), 1.0),
        "c_ctx": nrm(ks[3], (D_MODEL,), 1.0),
        "ada_w": nrm(ks[4], (DEPTH, D_MODEL, N_MOD * D_MODEL), 0.5 * D_MODEL ** -0.5),
        "ada_b": nrm(ks[5], (DEPTH, N_MOD * D_MODEL), 0.02),
        "norm_mix_g": 1.0 + nrm(ks[6], (DEPTH, D_MODEL), 0.02),
        "w_in": nrm(ks[7], (DEPTH, D_MODEL, IN_PROJ_WIDTH), D_MODEL ** -0.5),
        "conv_w": nrm(ks[8], (DEPTH, CONV_WIDTH, RNN_WIDTH), CONV_WIDTH ** -0.5),
        "conv_b": nrm(ks[9], (DEPTH, RNN_WIDTH), 0.02),
        "lru_w_a": nrm(ks[10], (DEPTH, 2, RNN_HEADS, RNN_BLOCK, RNN_BLOCK), RNN_BLOCK ** -0.5),
        "lru_b_a": nrm(ks[11], (DEPTH, 2, RNN_HEADS, RNN_BLOCK), 0.02),
        "lru_w_x": nrm(ks[12], (DEPTH, 2, RNN_HEADS, RNN_BLOCK, RNN_BLOCK), RNN_BLOCK ** -0.5),
        "lru_b_x": nrm(ks[13], (DEPTH, 2, RNN_HEADS, RNN_BLOCK), 0.02),
        "lru_lambda": lru_lambda,
        "w_proj_rnn": nrm(ks[14], (DEPTH, RNN_WIDTH, D_MODEL), RNN_WIDTH ** -0.5),
        "w_proj_fourier": nrm(ks[15], (DEPTH, FOURIER_WIDTH, D_MODEL), FOURIER_WIDTH ** -0.5),
        "w_out": nrm(ks[16], (DEPTH, D_MODEL, D_MODEL), D_MODEL ** -0.5),
        "norm_ffn_g": 1.0 + nrm(ks[17], (DEPTH, D_MODEL), 0.02),
        "router_w": nrm(ks[18], (D_MODEL, N_EXPERTS), D_MODEL ** -0.5),
        "router_b": nrm(ks[19], (N_EXPERTS,), 0.01),
        "moe_w1": nrm(ks[21], (DEPTH, N_EXPERTS, D_MODEL, D_EXPERT), D_MODEL ** -0.5),
        "moe_w3": nrm(ks[22], (DEPTH, N_EXPERTS, D_MODEL, D_EXPERT), D_MODEL ** -0.5),
        "moe_w2": nrm(ks[23], (DEPTH, N_EXPERTS, D_EXPERT, D_MODEL), D_EXPERT ** -0.5),
        "final_norm_g": 1.0 + nrm(ks[24], (D_MODEL,), 0.02),
    }


def reference(x, c, ctx, c_ctx, ada_w, ada_b, norm_mix_g, w_in, conv_w, conv_b, lru_w_a, lru_b_a,
              lru_w_x, lru_b_x, lru_lambda, w_proj_rnn, w_proj_fourier, w_out, norm_ffn_g,
              router_w, router_b, moe_w1, moe_w3, moe_w2, final_norm_g):
    h_ctx = ctx
    n_b = x.shape[0]
    zeros = jnp.zeros((n_b, RNN_WIDTH), jnp.float32)
    for l in range(DEPTH):
        last = l == DEPTH - 1
        mod_x = (jax.nn.silu(c) @ ada_w[l] + ada_b[l])[:, None, :]
        mod_c = (jax.nn.silu(c_ctx) @ ada_w[l] + ada_b[l])[None, None, :]
        sh1_x, sc1_x, g1_x, sh2_x, sc2_x, g2_x = jnp.split(mod_x, N_MOD, axis=-1)
        sh1_c, sc1_c, g1_c, sh2_c, sc2_c, g2_c = jnp.split(mod_c, N_MOD, axis=-1)
        lru_f = (lru_w_a[l, 0], lru_b_a[l, 0], lru_w_x[l, 0], lru_b_x[l, 0], lru_lambda[l, 0])
        lru_b = (lru_w_a[l, 1], lru_b_a[l, 1], lru_w_x[l, 1], lru_b_x[l, 1], lru_lambda[l, 1])

        hc = modulate(rms_norm(h_ctx, norm_mix_g[l]), sh1_c, sc1_c)
        if last:
            state_f, state_b = context_scan_states(hc, w_in[l], conv_w[l], conv_b[l], lru_f, lru_b)
        else:
            out_c, state_f, state_b = mixer(hc, False, zeros, zeros, w_in[l], conv_w[l], conv_b[l],
                                            lru_f, lru_b, w_proj_rnn[l], w_proj_fourier[l], w_out[l])
            h_ctx = h_ctx + g1_c * out_c

        hx = modulate(rms_norm(x, norm_mix_g[l]), sh1_x, sc1_x)
        out_x, _, _ = mixer(hx, True, state_f, state_b, w_in[l], conv_w[l], conv_b[l],
                            lru_f, lru_b, w_proj_rnn[l], w_proj_fourier[l], w_out[l])
        x = x + g1_x * out_x

        if not last:
            hc2 = modulate(rms_norm(h_ctx, norm_ffn_g[l]), sh2_c, sc2_c)
            h_ctx = h_ctx + g2_c * grouped_moe(hc2, router_w, router_b, moe_w1[l], moe_w3[l], moe_w2[l])
        hx2 = modulate(rms_norm(x, norm_ffn_g[l]), sh2_x, sc2_x)
        x = x + g2_x * grouped_moe(hx2, router_w, router_b, moe_w1[l], moe_w3[l], moe_w2[l])

    return rms_norm(x, final_norm_g)
```

```python
import numpy as np
import ml_dtypes
from contextlib import ExitStack
import concourse.bass as bass
import concourse.mybir as mybir
from concourse.bass_utils import run_bass_kernel_spmd

F32 = mybir.dt.float32
BF16 = mybir.dt.bfloat16
I32 = mybir.dt.int32
IOA = bass.IndirectOffsetOnAxis
NSLOT = 32 * 512
AF = mybir.ActivationFunctionType
ALU = mybir.AluOpType
AX = mybir.AxisListType

D = 1024
L = 4096
LC = 256
DEPTH = 2
NE = 16
EPS = 1e-6
SEM_LIMIT = 500
BIG = 1.0e9


class Buf:
    __slots__ = ("name", "w", "r", "psum")

    def __init__(self, name="", psum=False):
        self.name = name
        self.w = None
        self.r = {}
        self.psum = psum


class EngQ:
    def __init__(self, kb, eng, name, is_pe=False):
        self.kb = kb
        self.eng = eng
        self.name = name
        self.is_pe = is_pe
        self.cur = None
        self.cnt = 0
        self.waited = {}
        self.last = None

    def wait(self, tok):
        if tok is None or tok[3] != self.kb.epoch:
            return
        sem, c, key = tok[0], tok[1], tok[2]
        if self.waited.get(key, 0) >= c:
            return
        self.eng.wait_ge(sem, c)
        self.waited[key] = c

    def tick(self, inst):
        if self.cur is None or self.cnt >= SEM_LIMIT:
            self.cur = self.kb.new_sem(self.name)
            self.cnt = 0
        self.cnt += 1
        inst.then_inc(self.cur[0], 1)
        if self.kb.rec is not None:
            self.kb.rec_inc(self, self.cur[0], self.cur[1], 1, self.cnt - 1)
        self.last = (self.cur[0], self.cnt, self.cur[1], self.kb.epoch)
        return self.last


class KB:
    def __init__(self, dump=False, stop_after=None):
        self.nc = bass.Bass("TRN2", target_bir_lowering=False)
        self.dump = dump
        self.stop_after = stop_after
        self.es = ExitStack()
        self.nsem = 0
        nc = self.nc
        self.pe = EngQ(self, nc.tensor, "pe", is_pe=True)
        self.act = EngQ(self, nc.scalar, "act")
        self.dve = EngQ(self, nc.vector, "dve")
        self.pool = EngQ(self, nc.gpsimd, "pool")
        self.sp = EngQ(self, nc.sync, "sp")
        self.engs = [self.pe, self.act, self.dve, self.pool, self.sp]
        self.dma_sems = []
        self.dma_rr = 0
        self.dma_live = {}
        self.uid = 0
        self.pe_pending = {}
        self.epoch = 0
        self.my_sems = []
        self.rec = None

    def new_sem(self, name):
        self.nsem += 1
        s = self.nc.alloc_semaphore(name="s%s%d" % (name, self.nsem))
        self.my_sems.append(s)
        return (s, self.nsem)

    def dma_sem(self):
        if len(self.dma_sems) < 40:
            s, key = self.new_sem("dma")
            self.dma_sems.append([s, key, 0])
            ent = self.dma_sems[-1]
        else:
            i = self.dma_rr % 40
            if self.dma_sems[i][2] >= 30 * 16:
                s, key = self.new_sem("dma")
                self.dma_sems[i] = [s, key, 0]
            ent = self.dma_sems[i]
        self.dma_rr += 1
        return ent

    def op(self, q, fn, reads=(), writes=(), tick=True):
        for b in reads:
            q.wait(b.w)
            if b.psum:
                for t in b.r.values():
                    q.wait(t)
        for b in writes:
            assert q.is_pe or id(b) not in self.pe_pending, "write to buffer with un-ticked PE reads: %s" % b.name
            if not q.is_pe:
                q.wait(b.w)
            elif b.w is not None and not self._is_pe_tok(b.w):
                q.wait(b.w)
            for t in b.r.values():
                q.wait(t)
        inst = fn()
        if not tick:
            assert q.is_pe
            for b in reads:
                self.pe_pending[id(b)] = b
            for b in writes:
                b.r = {}
            return None
        tok = q.tick(inst)
        if q.is_pe and self.pe_pending:
            for b in self.pe_pending.values():
                b.r[tok[2]] = tok
            self.pe_pending = {}
        for b in reads:
            b.r[tok[2]] = tok
        for b in writes:
            b.w = tok
            b.r = {}
        return tok

    def _is_pe_tok(self, tok):
        return tok[2] in self.pe_keys

    @property
    def pe_keys(self):
        if not hasattr(self, "_pe_keys"):
            self._pe_keys = set()
        return self._pe_keys

    def pe_op(self, fn, reads=(), writes=(), tick=True):
        tok = self.op(self.pe, fn, reads, writes, tick=tick)
        if tok is not None:
            self.pe_keys.add(tok[2])
        return tok

    def dma(self, q, out, in_, reads=(), writes=(), **kw):
        ent = self.dma_sem()
        sem, key, tot = ent
        if tot > 0:
            q.wait((sem, tot, key, self.epoch))
        for b in reads:
            q.wait(b.w)
        for b in writes:
            assert id(b) not in self.pe_pending, "dma write to buffer with un-ticked PE reads: %s" % b.name
            q.wait(b.w)
            for t in b.r.values():
                q.wait(t)
        q.eng.dma_start(out=out, in_=in_, **kw).then_inc(sem, 16)
        if self.rec is not None:
            self.rec_inc(q, sem, key, 16, tot)
        ent[2] = tot + 16
        tok = (sem, tot + 16, key, self.epoch)
        self.dma_live[key] = tok
        for b in reads:
            b.r[key] = tok
        for b in writes:
            b.w = tok
            b.r = {}
        return tok

    def idma(self, out, out_off, in_, in_off, reads=(), writes=()):
        q = self.pool
        ent = self.dma_sem()
        sem, key, tot = ent
        if tot > 0:
            q.wait((sem, tot, key, self.epoch))
        for b in reads:
            q.wait(b.w)
        for b in writes:
            assert id(b) not in self.pe_pending, "dma write to buffer with un-ticked PE reads: %s" % b.name
            q.wait(b.w)
            for t in b.r.values():
                q.wait(t)
        self.nc.gpsimd.indirect_dma_start(out=out, out_offset=out_off, in_=in_, in_offset=in_off).then_inc(sem, 16)
        ent[2] = tot + 16
        tok = (sem, tot + 16, key, self.epoch)
        self.dma_live[key] = tok
        for b in reads:
            b.r[key] = tok
        for b in writes:
            b.w = tok
            b.r = {}
        return tok

    def rec_inc(self, q, sem, key, amount, before):
        d = self.rec.setdefault(q.name, {})
        if key not in d:
            d[key] = [sem, 0, before]
        d[key][1] += amount

    def cond_block(self, cnt_regs, thr, body, engines=None):
        engines = engines or self.engs
        assert self.rec is None and not self.pe_pending
        self.rec = {}
        with self.nc.If(cnt_regs > thr):
            body()
            assert not self.pe_pending
        rec, self.rec = self.rec, None
        with self.nc.Else():
            for q in self.engs:
                incs = rec.get(q.name, {})
                for key, (sem, amount, before) in incs.items():
                    if before > 0:
                        q.eng.wait_ge(sem, before)
                    left = amount
                    while left > 0:
                        a = min(left, 16)
                        q.eng.nop().then_inc(sem, a)
                        left -= a
        for q in self.engs:
            q.waited = {}

    def load_count_regs(self, ap, engines=None):
        return self.nc.values_load(ap, min_val=0, max_val=16384)

    def barrier(self):
        toks = [q.last for q in self.engs if q.last is not None] + list(self.dma_live.values())
        for q in self.engs:
            for t in toks:
                q.wait(t)
        assert not self.pe_pending
        self.nc.all_engine_barrier()
        self.nc.clear_and_free_semaphores(list(self.my_sems))
        self.nc.all_engine_barrier()
        self.my_sems = []
        self.epoch += 1
        for q in self.engs:
            q.cur, q.cnt, q.waited, q.last = None, 0, {}, None
        self.dma_sems, self.dma_rr, self.dma_live = [], 0, {}
        self._pe_keys = set()

    def sb(self, st, name, shape, dtype):
        self.uid += 1
        return st.enter_context(self.nc.sbuf_tensor("%s_%d" % (name, self.uid), list(shape), dtype))

    def dram(self, name, shape, dtype, dumpable=True):
        kind = "ExternalOutput" if (self.dump and dumpable) else "Internal"
        return self.nc.dram_tensor(name, list(shape), dtype, kind=kind).ap()


def bc_last(ap, n):
    pat = [list(x) for x in ap.ap] + [[0, n]]
    return bass.AP(ap.tensor, ap.offset, pat)


def bc_mid(ap, n):
    pat = [list(x) for x in ap.ap]
    return bass.AP(ap.tensor, ap.offset, [pat[0], [0, n]] + pat[1:])


def rev_last(ap):
    pat = [list(x) for x in ap.ap]
    step, n = pat[-1]
    pat[-1] = [-step, n]
    return bass.AP(ap.tensor, ap.offset + step * (n - 1), pat)


class Stream:
    pass


def build_program(dump=False, stop_after=None):
    kb = KB(dump=dump, stop_after=stop_after)
    nc = kb.nc
    pe, act, dve, pool, sp = kb.pe, kb.act, kb.dve, kb.pool, kb.sp

    def din(name, shape, dtype=F32):
        return nc.dram_tensor(name, list(shape), dtype, kind="ExternalInput").ap()

    x_in = din("x", [L, D])
    ctx_in = din("ctx", [LC, D])
    cc_in = din("cc", [2, D])
    ada_w = din("ada_w", [DEPTH, D, 6 * D])
    ada_b = din("ada_b", [DEPTH, 6 * D])
    norm_mix_g = din("norm_mix_g", [DEPTH, D])
    w_in = din("w_in", [DEPTH, D, 4608])
    conv_w = din("conv_w", [DEPTH, 4, D])
    conv_b = din("conv_b", [DEPTH, D])
    lru_w_a = din("lru_w_a", [DEPTH, 2, 8, 128, 128])
    lru_b_a = din("lru_b_a", [DEPTH, 2, 8, 128])
    lru_w_x = din("lru_w_x", [DEPTH, 2, 8, 128, 128])
    lru_b_x = din("lru_b_x", [DEPTH, 2, 8, 128])
    lru_lambda = din("lru_lambda", [DEPTH, 2, D])
    w_proj_rnn = din("w_proj_rnn", [DEPTH, D, D])
    w_proj_fourier = din("w_proj_fourier", [DEPTH, 512, D])
    w_out = din("w_out", [DEPTH, D, D])
    norm_ffn_g = din("norm_ffn_g", [DEPTH, D])
    router_w = din("router_w", [D, NE])
    router_b = din("router_b", [1, NE])
    moe_w1 = din("moe_w1", [DEPTH, NE, D, D])
    moe_w3 = din("moe_w3", [DEPTH, NE, D, D])
    moe_w2 = din("moe_w2", [DEPTH, NE, D, D])
    final_norm_g = din("final_norm_g", [1, D])
    ident_in = din("ident", [128, 128])
    csc_in = din("csc", [128, 256])
    rc_in = din("rconst", [128, 448])
    dftl_in = din("dftl", [2, L, L], BF16)
    dftc_in = din("dftc", [2, LC, LC], BF16)
    out_d = nc.dram_tensor("out", [L, D], F32, kind="ExternalOutput").ap()

    def mk_stream(name, Ln, T, on_grid, src):
        s = Stream()
        s.name, s.L, s.T, s.on_grid, s.src = name, Ln, T, on_grid, src
        s.nt = Ln // T
        s.nsub = T // 128
        s.xres = kb.dram("xres_" + name, [Ln, D], F32)
        s.us = kb.dram("us_" + name, [128, 8, Ln], F32)
        s.hf = kb.dram("hf_" + name, [128, 8, Ln], F32)
        s.hxT = kb.dram("hxT_" + name, [128, 8, Ln], BF16)
        s.AB = kb.dram("AB_" + name, [Ln, D], BF16)
        s.yfT = kb.dram("yfT_" + name, [128, 4, Ln], BF16)
        s.b_xres = [Buf() for _ in range(s.nt)]
        s.b_us = [Buf() for _ in range(s.nt)]
        s.b_hf = [Buf() for _ in range(s.nt)]
        s.b_hxT = [Buf() for _ in range(s.nt)]
        s.b_AB = [Buf() for _ in range(s.nt)]
        s.b_yfT = [Buf() for _ in range(s.nt)]
        s.first = True
        return s

    SX = mk_stream("x", L, 512, True, x_in)
    SC = mk_stream("c", LC, 256, False, ctx_in)
    SX.sidx, SC.sidx = 0, 1
    SX.dft, SC.dft = dftl_in, dftc_in
    LT = LC + L
    h2s_d = kb.dram("h2s", [NSLOT, D], BF16)
    ys_d = kb.dram("ys", [NSLOT, D], F32)
    wbf_d = [[kb.dram("wbf_%d_%d" % (l_, m_), [NE * 128, 8 * D], BF16, dumpable=False) for m_ in range(3)]
             for l_ in range(DEPTH)]
    cast_q = {l_: [(m_, e_) for e_ in range(NE) for m_ in range(3)] for l_ in range(DEPTH)}
    cast_l = [0]

    def feed(n=2):
        q_ = cast_q[cast_l[0]]
        wsrc_ = (moe_w1, moe_w3, moe_w2)
        for _ in range(n):
            if not q_:
                return
            m_, e_ = q_.pop(0)
            kb.dma(pool, wbf_d[cast_l[0]][m_][e_ * 128:(e_ + 1) * 128, :],
                   wsrc_[m_][cast_l[0], e_].rearrange("(p c) f -> p (c f)", c=8))

    top = kb.es
    ident = kb.sb(top, "ident", [128, 128], F32)
    b_ident = Buf()
    kb.dma(sp, ident[:], ident_in, writes=[b_ident])
    csc = kb.sb(top, "csc", [128, 256], BF16)
    b_csc = Buf()
    kb.dma(pool, csc[:], csc_in, writes=[b_csc])
    ones = kb.sb(top, "ones", [128, 128], F32)
    b_ones = Buf()
    kb.op(dve, lambda: nc.vector.memset(ones[:], 1.0), writes=[b_ones])
    ccf = kb.sb(top, "ccf", [128, 8, 2], F32)
    b_ccf = Buf()
    with nc.allow_non_contiguous_dma(reason="tiny one-time transposed load"):
        for s in range(2):
            kb.dma(sp, ccf[:, :, s], cc_in[s].rearrange("(c p) -> p c", p=128), writes=[b_ccf])
    kb.op(act, lambda: nc.scalar.activation(out=ccf[:], in_=ccf[:], func=AF.Silu), reads=[b_ccf], writes=[b_ccf])
    rw = kb.sb(top, "rw", [128, 8, 128], F32)
    b_rw = Buf()
    kb.op(dve, lambda: nc.vector.memset(rw[:].rearrange("p c n -> p (c n)"), 0.0), writes=[b_rw])
    kb.dma(sp, rw[:, :, :NE], router_w.rearrange("(c p) n -> p c n", p=128), writes=[b_rw])
    rb_bc = kb.sb(top, "rb_bc", [128, NE], F32)
    b_rb = Buf()
    kb.dma(sp, rb_bc[:], router_b.partition_broadcast(128), writes=[b_rb])
    gF_bc = kb.sb(top, "gF_bc", [128, D], F32)
    b_gF = Buf()
    kb.dma(sp, gF_bc[:], final_norm_g.partition_broadcast(128), writes=[b_gF])
    carry = kb.sb(top, "carry", [128, 2, 8], F32)
    b_carry = [[Buf() for _ in range(8)] for _ in range(2)]
    g2 = kb.sb(top, "g2", [128, 2, D], F32)
    b_g2 = [Buf(), Buf()]
    NSMAX = 34
    rc = kb.sb(top, "rc", [128, 448], F32)
    b_rc = Buf()
    kb.dma(sp, rc[:], rc_in, writes=[b_rc])
    identb = kb.sb(top, "identb", [128, 128], BF16)
    b_identb = Buf()
    kb.op(dve, lambda: nc.vector.tensor_copy(out=identb[:], in_=ident[:]), reads=[b_ident], writes=[b_identb])
    zrow = kb.sb(top, "zrow", [128, D], BF16)
    b_zrow = Buf()
    kb.op(dve, lambda: nc.vector.memset(zrow[:], 0.0), writes=[b_zrow])
    pos2i = kb.sb(top, "pos2i", [128, NSMAX, 2], I32)
    gate2 = kb.sb(top, "gate2", [128, NSMAX, 2], F32)
    widx = kb.sb(top, "widx", [128, 32], I32)
    b_route = Buf()

    banks = []
    for i in range(8):
        t = top.enter_context(nc.psum_tensor("bank%d" % i, [128, 512], F32))
        banks.append((t, Buf("bank%d" % i, psum=True)))
    bank_rr = [0]

    bank_mode = ["all"]
    bank_rr2 = [0]

    def next_bank():
        if bank_mode[0] == "hi":
            b = banks[4 + bank_rr2[0] % 4]
            bank_rr2[0] += 1
            return b
        b = banks[bank_rr[0] % 8]
        bank_rr[0] += 1
        return b

    def mm(bank, out_ap, lhsT, rhs, reads, start, stop, tick=None):
        return kb.pe_op(lambda: nc.tensor.matmul(out_ap, lhsT, rhs, start=start, stop=stop),
                        reads=reads, writes=[bank[1]], tick=(stop if tick is None else tick))

    def mod_phase(l, part, mods, b_mods, gnorm_dram):
        with ExitStack() as st:
            crep = kb.sb(st, "crep", [128, 2, 8, 128], F32)
            b_crep = Buf()
            for s in range(2):
                for c in range(8):
                    kb.op(dve, lambda s=s, c=c: nc.vector.tensor_scalar(
                        out=crep[:, s, c, :], in0=ones[:], scalar1=ccf[:, c, s:s + 1], scalar2=None, op0=ALU.mult),
                        reads=[b_ones, b_ccf], writes=[b_crep])
            adab = kb.sb(st, "adab", [128, 3 * D], F32)
            b_adab = Buf()
            kb.dma(sp, adab[:], ada_b[l:l + 1, part * 3 * D:(part + 1) * 3 * D].partition_broadcast(128),
                   writes=[b_adab])
            gn = kb.sb(st, "gn", [128, D], F32)
            b_gn = Buf()
            kb.dma(sp, gn[:], gnorm_dram[l:l + 1, :].partition_broadcast(128), writes=[b_gn])
            wblk = [kb.sb(st, "wblk", [128, 8, 512], F32) for _ in range(2)]
            b_wblk = [Buf(), Buf()]
            aw = ada_w[l].rearrange("(c p) n -> p c n", p=128)
            for blk in range(6):
                col0 = part * 3 * D + blk * 512
                wb, bwb = wblk[blk % 2], b_wblk[blk % 2]
                kb.dma(sp, wb[:], aw[:, :, col0:col0 + 512], writes=[bwb])
                j, off = blk // 2, (blk % 2) * 512
                for s in range(2):
                    bank = next_bank()
                    for k in range(8):
                        mm(bank, bank[0][:, :], crep[:, s, k, :], wb[:, k, :], [b_crep, bwb], k == 0, k == 7)
                    kb.op(dve, lambda s=s, j=j, off=off, bank=bank, blk=blk: nc.vector.tensor_tensor(
                        out=mods[:, s, j, off:off + 512], in0=bank[0][:, :], in1=adab[:, blk * 512:(blk + 1) * 512],
                        op=ALU.add), reads=[bank[1], b_adab], writes=[b_mods[s][j]])
            for s in range(2):
                kb.op(dve, lambda s=s: nc.vector.scalar_tensor_tensor(
                    out=mods[:, s, 1, :], in0=mods[:, s, 1, :], scalar=1.0, in1=gn[:], op0=ALU.add, op1=ALU.mult),
                    reads=[b_gn, b_mods[s][1]], writes=[b_mods[s][1]])
            kb.barrier()

    def load_x_sub(S, ti, s, xt, b_xt):
        r0 = ti * S.T + s * 128
        src = S.src if S.first else S.xres
        kb.dma(sp, xt[:], src[r0:r0 + 128, :], reads=([] if S.first else [S.b_xres[ti]]), writes=[b_xt])

    def norm_sub(xt, b_xt, hx, b_hx, Gt, b_G, St, b_S, sc, b_sc):
        kb.op(act, lambda: nc.scalar.activation(out=hx[:], in_=xt[:], func=AF.Square, accum_out=sc),
              reads=[b_xt], writes=[b_hx, b_sc])
        kb.op(dve, lambda: nc.vector.tensor_scalar(out=sc, in0=sc, scalar1=1.0 / D, scalar2=EPS, op0=ALU.mult,
                                                   op1=ALU.add), reads=[b_sc], writes=[b_sc])
        kb.op(act, lambda: nc.scalar.activation(out=sc, in_=sc, func=AF.Ln), reads=[b_sc], writes=[b_sc])
        kb.op(act, lambda: nc.scalar.activation(out=sc, in_=sc, func=AF.Exp, scale=-0.5), reads=[b_sc], writes=[b_sc])
        kb.op(dve, lambda: nc.vector.scalar_tensor_tensor(out=hx[:], in0=xt[:], scalar=sc, in1=Gt, op0=ALU.mult,
                                                          op1=ALU.mult), reads=[b_xt, b_sc, b_G], writes=[b_hx])
        kb.op(dve, lambda: nc.vector.tensor_tensor(out=hx[:], in0=hx[:], in1=St, op=ALU.add),
              reads=[b_S, b_hx], writes=[b_hx])

    def transpose_sub(hx, b_hx, s, outs):
        for half in range(2):
            bank = next_bank()
            for cc in range(4):
                c = half * 4 + cc
                kb.pe_op(lambda c=c, cc=cc, bank=bank: nc.tensor.transpose(
                    out=bank[0][:, cc * 128:(cc + 1) * 128], in_=hx[:, c * 128:(c + 1) * 128], identity=ident[:]),
                    reads=[b_hx, b_ident], writes=[bank[1]], tick=(cc == 3))
            pv = bank[0][:, :].rearrange("p (c t) -> p c t", t=128)
            srcv, srcb = pv, [bank[1]]
            for (q, tl, bufs) in outs:
                dst = tl[:, half * 4:(half + 1) * 4, s * 128:(s + 1) * 128]
                wb = bufs[half * 4:(half + 1) * 4]
                if q is act:
                    kb.op(act, lambda dst=dst, srcv=srcv: nc.scalar.copy(out=dst, in_=srcv), reads=srcb, writes=wb)
                else:
                    kb.op(dve, lambda dst=dst, srcv=srcv: nc.vector.tensor_copy(out=dst, in_=srcv), reads=srcb,
                          writes=wb)
                srcv, srcb = dst, wb

    def norm_transpose_tile(S, ti, wkn, G, bG, Sh, bSh, outs, tok_out=None):
        xts, b_xts, hxs, b_hxs, ss, b_ss, cnt = wkn
        for s0 in range(0, S.nsub, 2):
            pr = [(s0 + j, j) for j in range(2) if s0 + j < S.nsub]
            for (s, i) in pr:
                load_x_sub(S, ti, s, xts[i], b_xts[i])
            sc = lambda i: ss[:, i:i + 1]
            for (s, i) in pr:
                kb.op(act, lambda i=i: nc.scalar.activation(out=hxs[i][:], in_=xts[i][:], func=AF.Square,
                                                            accum_out=sc(i)), reads=[b_xts[i]],
                      writes=[b_hxs[i], b_ss[i]])
            for (s, i) in pr:
                kb.op(dve, lambda i=i: nc.vector.tensor_scalar(out=sc(i), in0=sc(i), scalar1=1.0 / D, scalar2=EPS,
                                                               op0=ALU.mult, op1=ALU.add), reads=[b_ss[i]],
                      writes=[b_ss[i]])
            for (s, i) in pr:
                kb.op(act, lambda i=i: nc.scalar.activation(out=sc(i), in_=sc(i), func=AF.Ln), reads=[b_ss[i]],
                      writes=[b_ss[i]])
            for (s, i) in pr:
                kb.op(act, lambda i=i: nc.scalar.activation(out=sc(i), in_=sc(i), func=AF.Exp, scale=-0.5),
                      reads=[b_ss[i]], writes=[b_ss[i]])
            for (s, i) in pr:
                kb.op(dve, lambda i=i: nc.vector.scalar_tensor_tensor(out=hxs[i][:], in0=xts[i][:], scalar=sc(i),
                                                                      in1=G, op0=ALU.mult, op1=ALU.mult),
                      reads=[b_xts[i], b_ss[i], bG], writes=[b_hxs[i]])
            for (s, i) in pr:
                kb.op(dve, lambda i=i: nc.vector.tensor_tensor(out=hxs[i][:], in0=hxs[i][:], in1=Sh, op=ALU.add),
                      reads=[bSh, b_hxs[i]], writes=[b_hxs[i]])
            if tok_out is not None:
                tk, b_tk, sbase = tok_out
                for (s, i) in pr:
                    kb.op(act, lambda s=s, i=i: nc.scalar.copy(out=tk[:, sbase + s, :], in_=hxs[i][:]),
                          reads=[b_hxs[i]], writes=[b_tk[sbase + s]])
            for (s, i) in pr:
                transpose_sub(hxs[i], b_hxs[i], s, outs)

    def norm_work(st):
        xts = [kb.sb(st, "xts", [128, D], F32) for _ in range(2)]
        hxs = [kb.sb(st, "hxs", [128, D], F32) for _ in range(2)]
        ss = kb.sb(st, "ss", [128, 2], F32)
        return (xts, [Buf(), Buf()], hxs, [Buf(), Buf()], ss, [Buf(), Buf()], [0])

    def scan_dir(S, d, T, lw, u, b_u, ubf, b_ubf, h, b_h, gk, reverse):
        lruw, b_lruw, lb, b_lb, nls, b_nls = lw
        r_all, i_all, a_all, b_r, b_i, b_a = gk
        A = lambda fn, rd, wr: kb.op(act, fn, reads=rd, writes=wr)
        V = lambda fn, rd, wr: kb.op(dve, fn, reads=rd, writes=wr)
        G = 4
        for c0 in range(0, 8, G):
            cs = list(range(c0, c0 + G))
            bks = {}
            for c in cs:
                bk_r = next_bank()
                mm(bk_r, bk_r[0][:, :T], lruw[:, d, 0, c, :], ubf[:, c, :T], [b_lruw, b_ubf[c]], True, True)
                bk_i = next_bank()
                mm(bk_i, bk_i[0][:, :T], lruw[:, d, 1, c, :], ubf[:, c, :T], [b_lruw, b_ubf[c]], True, True)
                bks[c] = (bk_r, bk_i)
            R = lambda c: r_all[:, c, :T]
            I = lambda c: i_all[:, c, :T]
            AA = lambda c: a_all[:, c, :T]
            for c in cs:
                A(lambda c=c: nc.scalar.activation(out=R(c), in_=bks[c][0][0][:, :T], func=AF.Exp, scale=-1.0,
                                                   bias=lb[:, d, 0, c:c + 1]), [bks[c][0][1], b_lb], [b_r[c]])
                A(lambda c=c: nc.scalar.activation(out=I(c), in_=bks[c][1][0][:, :T], func=AF.Exp, scale=-1.0,
                                                   bias=lb[:, d, 1, c:c + 1]), [bks[c][1][1], b_lb], [b_i[c]])
            for c in cs:
                A(lambda c=c: nc.scalar.activation(out=R(c), in_=R(c), func=AF.Ln, bias=1.0), [b_r[c]], [b_r[c]])
                A(lambda c=c: nc.scalar.activation(out=I(c), in_=I(c), func=AF.Ln, bias=1.0), [b_i[c]], [b_i[c]])
            for c in cs:
                A(lambda c=c: nc.scalar.activation(out=R(c), in_=R(c), func=AF.Exp, scale=-1.0), [b_r[c]], [b_r[c]])
                A(lambda c=c: nc.scalar.activation(out=I(c), in_=I(c), func=AF.Exp, scale=-1.0), [b_i[c]], [b_i[c]])
            for c in cs:
                A(lambda c=c: nc.scalar.activation(out=AA(c), in_=R(c), func=AF.Exp, scale=nls[:, d, 0, c:c + 1]),
                  [b_r[c], b_nls], [b_a[c]])
            for c in cs:
                A(lambda c=c: nc.scalar.activation(out=R(c), in_=R(c), func=AF.Exp, scale=nls[:, d, 1, c:c + 1]),
                  [b_r[c], b_nls], [b_r[c]])
                V(lambda c=c: nc.vector.tensor_tensor(out=I(c), in0=I(c), in1=u[:, c, :T], op=ALU.mult),
                  [b_i[c], b_u[c]], [b_i[c]])
            for c in cs:
                A(lambda c=c: nc.scalar.activation(out=R(c), in_=R(c), func=AF.Ln, scale=-1.0, bias=1.0),
                  [b_r[c]], [b_r[c]])
            for c in cs:
                A(lambda c=c: nc.scalar.activation(out=R(c), in_=R(c), func=AF.Exp, scale=0.5), [b_r[c]], [b_r[c]])
            for c in cs:
                V(lambda c=c: nc.vector.tensor_tensor(out=I(c), in0=I(c), in1=R(c), op=ALU.mult),
                  [b_i[c], b_r[c]], [b_i[c]])
            for c in cs:
                o_ap, a_ap, b_ap = h[:, c, :T], AA(c), I(c)
                if reverse:
                    o_ap, a_ap, b_ap = rev_last(o_ap), rev_last(a_ap), rev_last(b_ap)
                V(lambda c=c, o_ap=o_ap, a_ap=a_ap, b_ap=b_ap: nc.vector.tensor_tensor_scan(
                    out=o_ap, data0=a_ap, data1=b_ap, initial=carry[:, d, c:c + 1], op0=ALU.mult, op1=ALU.add),
                    [b_a[c], b_i[c], b_carry[d][c]], [b_h[c]])
            lastcol = 0 if reverse else T - 1
            for c in cs:
                V(lambda c=c: nc.vector.tensor_copy(out=carry[:, d, c:c + 1], in_=h[:, c, lastcol:lastcol + 1]),
                  [b_h[c]], [b_carry[d][c]])

    def load_lru(st, l):
        lruw = kb.sb(st, "lruw", [128, 2, 2, 8, 128], BF16)
        b_lruw = Buf()
        for d in range(2):
            kb.dma(pool, lruw[:, d, 0], lru_w_a[l, d].rearrange("h i j -> i h j"), writes=[b_lruw])
            kb.dma(pool, lruw[:, d, 1], lru_w_x[l, d].rearrange("h i j -> i h j"), writes=[b_lruw])
        lb = kb.sb(st, "lb", [128, 2, 2, 8], F32)
        b_lb = Buf()
        nls = kb.sb(st, "nls", [128, 2, 2, 8], F32)
        b_nls = Buf()
        with nc.allow_non_contiguous_dma(reason="tiny per-partition parameter loads"):
            for d in range(2):
                kb.dma(sp, lb[:, d, 0, :], lru_b_a[l, d].rearrange("h j -> j h"), writes=[b_lb])
                kb.dma(sp, lb[:, d, 1, :], lru_b_x[l, d].rearrange("h j -> j h"), writes=[b_lb])
                kb.dma(sp, nls[:, d, 0, :], lru_lambda[l, d].rearrange("(h j) -> j h", j=128), writes=[b_nls])
        kb.op(dve, lambda: nc.vector.tensor_scalar(out=lb[:].rearrange("p a b c -> p (a b c)"),
                                                   in0=lb[:].rearrange("p a b c -> p (a b c)"), scalar1=-1.0,
                                                   scalar2=None, op0=ALU.mult), reads=[b_lb], writes=[b_lb])
        for d in range(2):
            kb.op(act, lambda d=d: nc.scalar.activation(out=nls[:, d, 0, :], in_=nls[:, d, 0, :], func=AF.Exp,
                                                        scale=-1.0), reads=[b_nls], writes=[b_nls])
        for d in range(2):
            kb.op(act, lambda d=d: nc.scalar.activation(out=nls[:, d, 0, :], in_=nls[:, d, 0, :], func=AF.Ln,
                                                        bias=1.0), reads=[b_nls], writes=[b_nls])
        for d in range(2):
            kb.op(dve, lambda d=d: nc.vector.tensor_scalar(out=nls[:, d, 1, :], in0=nls[:, d, 0, :], scalar1=-16.0,
                                                           scalar2=None, op0=ALU.mult), reads=[b_nls], writes=[b_nls])
            kb.op(dve, lambda d=d: nc.vector.tensor_scalar(out=nls[:, d, 0, :], in0=nls[:, d, 0, :], scalar1=-8.0,
                                                           scalar2=None, op0=ALU.mult), reads=[b_nls], writes=[b_nls])
        return (lruw, b_lruw, lb, b_lb, nls, b_nls)

    def gate_work(st, Tw=512):
        r_all = kb.sb(st, "r_all", [128, 8, Tw], F32)
        i_all = kb.sb(st, "i_all", [128, 8, Tw], F32)
        a_all = kb.sb(st, "a_all", [128, 8, Tw], F32)
        return (r_all, i_all, a_all, [Buf() for _ in range(8)], [Buf() for _ in range(8)],
                [Buf() for _ in range(8)])

    def sweep1(S, l, mods, b_mods, do_four):
        T, nsub = S.T, S.nsub
        with ExitStack() as st:
            win1 = kb.sb(st, "win1", [128, 8, 1536], BF16)
            b_win1 = Buf()
            wv = w_in[l].rearrange("(c p) n -> p c n", p=128)
            kb.dma(pool, win1[:, :, 0:1024], wv[:, :, 0:1024], writes=[b_win1])
            kb.dma(pool, win1[:, :, 1024:1536], wv[:, :, 2048:2560], writes=[b_win1])
            lw = load_lru(st, l)
            cw = kb.sb(st, "cw", [128, 8, 4], F32)
            cb = kb.sb(st, "cb", [128, 8], F32)
            b_cw = Buf()
            with nc.allow_non_contiguous_dma(reason="tiny per-partition parameter loads"):
                for k in range(4):
                    kb.dma(sp, cw[:, :, k], conv_w[l, k].rearrange("(c p) -> p c", p=128), writes=[b_cw])
                kb.dma(sp, cb[:], conv_b[l].rearrange("(c p) -> p c", p=128), writes=[b_cw])
            wkn = norm_work(st)
            hxT = kb.sb(st, "hxT", [128, 8, 512], BF16)
            b_hxT = [Buf() for _ in range(8)]
            u = kb.sb(st, "u", [128, 8, 512], F32)
            b_u = [Buf() for _ in range(8)]
            ubf = kb.sb(st, "ubf", [128, 8, 512], BF16)
            b_ubf = [Buf() for _ in range(8)]
            u4 = kb.sb(st, "u4", [128, 4, 512], BF16)
            b_u4 = [Buf() for _ in range(4)]
            abt = kb.sb(st, "abt", [128, 4, D], BF16)
            b_abt = [Buf() for _ in range(4)]
            gk = gate_work(st)
            hf, b_hf = gk[0], gk[3]
            G, bG, Sh, bSh = mods[:, S.sidx, 1, :], b_mods[S.sidx][1], mods[:, S.sidx, 0, :], b_mods[S.sidx][0]
            R = T // 64 if S.on_grid else 1
            W = 64 if S.on_grid else T
            for ti in range(S.nt):
                feed()
                norm_transpose_tile(S, ti, wkn, G, bG, Sh, bSh, [(act, hxT, b_hxT)])
                kb.dma(pool, S.hxT[:, :, ti * T:(ti + 1) * T], hxT[:, :, :T], reads=b_hxT, writes=[S.b_hxT[ti]])
                for c0 in range(0, 8, 4):
                    cs = list(range(c0, c0 + 4))
                    bk = {}
                    for c in cs:
                        bank = next_bank()
                        for k in range(8):
                            mm(bank, bank[0][:, :T], win1[:, k, c * 128:(c + 1) * 128], hxT[:, k, :T],
                               [b_win1, b_hxT[k]], k == 0, k == 7)
                        bk[c] = bank
                    PV = lambda c: bk[c][0][:, :T].rearrange("p (r w) -> p r w", w=W)
                    UV = lambda c: u[:, c, :T].rearrange("p (r w) -> p r w", w=W)
                    for c in cs:
                        kb.op(act, lambda c=c: nc.scalar.activation(
                            out=u[:, c, :T], in_=bk[c][0][:, :T], func=AF.Identity, scale=cw[:, c, 2:3],
                            bias=cb[:, c:c + 1]), reads=[bk[c][1], b_cw], writes=[b_u[c]])
                    for (k, so, do, n) in ((1, 0, 1, W - 1), (0, 0, 2, W - 2), (3, 1, 0, W - 1)):
                        for c in cs:
                            kb.op(dve, lambda c=c, k=k, so=so, do=do, n=n: nc.vector.scalar_tensor_tensor(
                                out=UV(c)[:, :, do:do + n], in0=PV(c)[:, :, so:so + n], scalar=cw[:, c, k:k + 1],
                                in1=UV(c)[:, :, do:do + n], op0=ALU.mult, op1=ALU.add),
                                reads=[bk[c][1], b_cw, b_u[c]], writes=[b_u[c]])
                    for c in cs:
                        kb.op(act, lambda c=c: nc.scalar.copy(out=ubf[:, c, :T], in_=u[:, c, :T]),
                              reads=[b_u[c]], writes=[b_ubf[c]])
                kb.dma(pool, S.us[:, :, ti * T:(ti + 1) * T], u[:, :, :T], reads=b_u, writes=[S.b_us[ti]])
                if do_four:
                    for g in range(4):
                        bank = next_bank()
                        for k in range(8):
                            mm(bank, bank[0][:, :T], win1[:, k, 1024 + g * 128:1024 + (g + 1) * 128], hxT[:, k, :T],
                               [b_win1, b_hxT[k]], k == 0, k == 7)
                        kb.op(act, lambda g=g, bank=bank: nc.scalar.copy(out=u4[:, g, :T], in_=bank[0][:, :T]),
                              reads=[bank[1]], writes=[b_u4[g]])
                    for s in range(nsub):
                        for g in range(4):
                            bank = next_bank()
                            mm(bank, bank[0][:, :256], u4[:, g, s * 128:(s + 1) * 128], csc[:, :],
                               [b_u4[g], b_csc], True, True)
                            kb.op(dve, lambda s=s, g=g, bank=bank: nc.vector.tensor_copy(
                                out=abt[:, s, g * 256:(g + 1) * 256], in_=bank[0][:, :256]),
                                reads=[bank[1]], writes=[b_abt[s]])
                    kb.dma(pool, S.AB[ti * T:(ti + 1) * T, :].rearrange("(s p) n -> p s n", p=128), abt[:, :nsub, :],
                           reads=b_abt[:nsub], writes=[S.b_AB[ti]])
                scan_dir(S, 0, T, lw, u, b_u, ubf, b_ubf, hf, b_hf, gk, False)
                kb.dma(pool, S.hf[:, :, ti * T:(ti + 1) * T], hf[:, :, :T], reads=b_hf, writes=[S.b_hf[ti]])
            kb.barrier()

    def sweep2a(S, l, need_h):
        T = S.T
        with ExitStack() as st:
            lw = load_lru(st, l)
            u = kb.sb(st, "u", [128, 8, 512], F32)
            b_u = [Buf() for _ in range(8)]
            b_uall = Buf()
            ubf = kb.sb(st, "ubf", [128, 8, 512], BF16)
            b_ubf = [Buf() for _ in range(8)]
            hf = kb.sb(st, "hft", [128, 8, 512], F32)
            b_hfa = Buf()
            gk = gate_work(st)
            hb, b_hb = gk[0], gk[3]
            for ti in reversed(range(S.nt)):
                kb.dma(sp, u[:, :, :T], S.us[:, :, ti * T:(ti + 1) * T], reads=[S.b_us[ti]], writes=b_u)
                for c in range(8):
                    kb.op(act, lambda c=c: nc.scalar.copy(out=ubf[:, c, :T], in_=u[:, c, :T]),
                          reads=[b_u[c]], writes=[b_ubf[c]])
                scan_dir(S, 1, T, lw, u, b_u, ubf, b_ubf, hb, b_hb, gk, True)
                if need_h:
                    kb.dma(sp, hf[:, :, :T], S.hf[:, :, ti * T:(ti + 1) * T], reads=[S.b_hf[ti]], writes=[b_hfa])
                    for c in range(8):
                        kb.op(dve, lambda c=c: nc.vector.tensor_tensor(out=hb[:, c, :T], in0=hb[:, c, :T],
                                                                       in1=hf[:, c, :T], op=ALU.add),
                              reads=[b_hfa, b_hb[c]], writes=[b_hb[c]])
                    kb.dma(pool, S.hf[:, :, ti * T:(ti + 1) * T], hb[:, :, :T], reads=b_hb, writes=[S.b_hf[ti]])
            kb.barrier()

    def dft_phase(S):
        nch = S.L // 128
        Tb = S.T
        nblk = S.L // Tb
        pc = min(8, nch)
        npiece = nch // pc
        with ExitStack() as st:
            ab = kb.sb(st, "ab_all", [128, nch, D], BF16)
            b_ab = Buf()
            for ti in range(S.nt):
                n0 = ti * S.nsub
                kb.dma(sp, ab[:, n0:n0 + S.nsub, :],
                       S.AB[ti * S.T:(ti + 1) * S.T, :].rearrange("(s p) n -> p s n", p=128),
                       reads=[S.b_AB[ti]], writes=[b_ab])
            tabs = [kb.sb(st, "tab", [128, pc, 2, Tb], BF16) for _ in range(3)]
            b_tabs = [Buf() for _ in range(3)]
            yft = [kb.sb(st, "yft", [128, 4, Tb], BF16) for _ in range(2)]
            b_yft = [Buf(), Buf()]
            tv = [S.dft[k].rearrange("(c p) n -> p c n", p=128) for k in range(2)]
            pi = 0
            for j in range(nblk):
                bks = [next_bank() for _ in range(4)]
                for q in range(npiece):
                    tab, b_tab = tabs[pi % 3], b_tabs[pi % 3]
                    pi += 1
                    for k in range(2):
                        kb.dma(sp, tab[:, :, k, :], tv[k][:, q * pc:(q + 1) * pc, j * Tb:(j + 1) * Tb],
                               writes=[b_tab])
                    for cc in range(pc):
                        c = q * pc + cc
                        for g in range(4):
                            mm(bks[g], bks[g][0][:, :Tb], ab[:, c, g * 256:g * 256 + 128], tab[:, cc, 0, :],
                               [b_ab, b_tab], c == 0, False)
                            mm(bks[g], bks[g][0][:, :Tb], ab[:, c, g * 256 + 128:g * 256 + 256], tab[:, cc, 1, :],
                               [b_ab, b_tab], False, c == nch - 1, tick=(c == nch - 1 or (cc == pc - 1 and g == 3)))
                y, b_y = yft[j % 2], b_yft[j % 2]
                for g in range(4):
                    kb.op(act, lambda g=g, y=y, bks=bks: nc.scalar.copy(out=y[:, g, :], in_=bks[g][0][:, :Tb]),
                          reads=[bks[g][1]], writes=[b_y])
                kb.dma(pool, S.yfT[:, :, j * Tb:(j + 1) * Tb], y[:, :, :], reads=[b_y], writes=[S.b_yfT[j]])
            kb.barrier()

    def scan_group(d, T, lw, cs, u_t, b_ut, ubf_t, b_ubft, gw, reverse):
        lruw, b_lruw, lb, b_lb, nls, b_nls = lw
        r_t, i_t, a_t, b_rt, b_it, b_at = gw
        A = lambda fn, rd, wr: kb.op(act, fn, reads=rd, writes=wr)
        V = lambda fn, rd, wr: kb.op(dve, fn, reads=rd, writes=wr)
        n = len(cs)
        bks = []
        for j, c in enumerate(cs):
            bk_r = next_bank()
            mm(bk_r, bk_r[0][:, :T], lruw[:, d, 0, c, :], ubf_t[:, j, :T], [b_lruw, b_ubft[j]], True, True)
            bk_i = next_bank()
            mm(bk_i, bk_i[0][:, :T], lruw[:, d, 1, c, :], ubf_t[:, j, :T], [b_lruw, b_ubft[j]], True, True)
            bks.append((bk_r, bk_i))
        R = lambda j: r_t[:, j, :T]
        I = lambda j: i_t[:, j, :T]
        AA = lambda j: a_t[:, j, :T]
        for j, c in enumerate(cs):
            A(lambda j=j, c=c: nc.scalar.activation(out=R(j), in_=bks[j][0][0][:, :T], func=AF.Exp, scale=-1.0,
                                                    bias=lb[:, d, 0, c:c + 1]), [bks[j][0][1], b_lb], [b_rt[j]])
            A(lambda j=j, c=c: nc.scalar.activation(out=I(j), in_=bks[j][1][0][:, :T], func=AF.Exp, scale=-1.0,
                                                    bias=lb[:, d, 1, c:c + 1]), [bks[j][1][1], b_lb], [b_it[j]])
        for j in range(n):
            A(lambda j=j: nc.scalar.activation(out=R(j), in_=R(j), func=AF.Ln, bias=1.0), [b_rt[j]], [b_rt[j]])
            A(lambda j=j: nc.scalar.activation(out=I(j), in_=I(j), func=AF.Ln, bias=1.0), [b_it[j]], [b_it[j]])
        for j in range(n):
            A(lambda j=j: nc.scalar.activation(out=R(j), in_=R(j), func=AF.Exp, scale=-1.0), [b_rt[j]], [b_rt[j]])
            A(lambda j=j: nc.scalar.activation(out=I(j), in_=I(j), func=AF.Exp, scale=-1.0), [b_it[j]], [b_it[j]])
        for j, c in enumerate(cs):
            A(lambda j=j, c=c: nc.scalar.activation(out=AA(j), in_=R(j), func=AF.Exp, scale=nls[:, d, 0, c:c + 1]),
              [b_rt[j], b_nls], [b_at[j]])
        for j, c in enumerate(cs):
            A(lambda j=j, c=c: nc.scalar.activation(out=R(j), in_=R(j), func=AF.Exp, scale=nls[:, d, 1, c:c + 1]),
              [b_rt[j], b_nls], [b_rt[j]])
            V(lambda j=j: nc.vector.tensor_tensor(out=I(j), in0=I(j), in1=u_t[:, j, :T], op=ALU.mult),
              [b_it[j], b_ut[j]], [b_it[j]])
        for j in range(n):
            A(lambda j=j: nc.scalar.activation(out=R(j), in_=R(j), func=AF.Ln, scale=-1.0, bias=1.0),
              [b_rt[j]], [b_rt[j]])
        for j in range(n):
            A(lambda j=j: nc.scalar.activation(out=R(j), in_=R(j), func=AF.Exp, scale=0.5), [b_rt[j]], [b_rt[j]])
        for j in range(n):
            V(lambda j=j: nc.vector.tensor_tensor(out=I(j), in0=I(j), in1=R(j), op=ALU.mult),
              [b_it[j], b_rt[j]], [b_it[j]])
        for j, c in enumerate(cs):
            o_ap, a_ap, b_ap = R(j), AA(j), I(j)
            if reverse:
                o_ap, a_ap, b_ap = rev_last(o_ap), rev_last(a_ap), rev_last(b_ap)
            V(lambda c=c, o_ap=o_ap, a_ap=a_ap, b_ap=b_ap: nc.vector.tensor_tensor_scan(
                out=o_ap, data0=a_ap, data1=b_ap, initial=carry[:, d, c:c + 1], op0=ALU.mult, op1=ALU.add),
                [b_at[j], b_it[j], b_carry[d][c]], [b_rt[j]])
        lastcol = 0 if reverse else T - 1
        for j, c in enumerate(cs):
            V(lambda j=j, c=c: nc.vector.tensor_copy(out=carry[:, d, c:c + 1], in_=r_t[:, j, lastcol:lastcol + 1]),
              [b_rt[j]], [b_carry[d][c]])

    def sweep2a_dft(S, l, need_h, do_dft):
        Th = 256
        nh = S.L // Th
        GC = 2
        with ExitStack() as st:
            lw = load_lru(st, l)
            NB = 2
            u_g = [kb.sb(st, "u_g", [128, GC, Th], F32) for _ in range(NB)]
            b_ug = [[Buf() for _ in range(GC)] for _ in range(NB)]
            ubf_g = [kb.sb(st, "ubf_g", [128, GC, Th], BF16) for _ in range(NB)]
            b_ubfg = [[Buf() for _ in range(GC)] for _ in range(NB)]
            hf_g = [kb.sb(st, "hf_g", [128, GC, Th], F32) for _ in range(NB)]
            b_hfg = [Buf() for _ in range(NB)]
            gws = []
            for _ in range(NB):
                gws.append((kb.sb(st, "r_g", [128, GC, Th], F32), kb.sb(st, "i_g", [128, GC, Th], F32),
                            kb.sb(st, "a_g", [128, GC, Th], F32), [Buf() for _ in range(GC)],
                            [Buf() for _ in range(GC)], [Buf() for _ in range(GC)]))
            groups = [(hi, c0) for hi in reversed(range(nh)) for c0 in range(0, 8, GC)]

            def g_pro(gi):
                hi, c0 = groups[gi]
                k = gi % NB
                ti = (hi * Th) // S.T
                cols = slice(hi * Th, (hi + 1) * Th)
                kb.dma(pool, u_g[k][:, :, :], S.us[:, c0:c0 + GC, cols], reads=[S.b_us[ti]], writes=b_ug[k])
                if need_h:
                    kb.dma(pool, hf_g[k][:, :, :], S.hf[:, c0:c0 + GC, cols], reads=[S.b_hf[ti]], writes=[b_hfg[k]])
                kb.dma(pool, ubf_g[k][:, :, :], S.us[:, c0:c0 + GC, cols], reads=[S.b_us[ti]], writes=b_ubfg[k])

            def g_main(gi):
                hi, c0 = groups[gi]
                k = gi % NB
                ti = (hi * Th) // S.T
                cols = slice(hi * Th, (hi + 1) * Th)
                gw = gws[k]
                scan_group(1, Th, lw, list(range(c0, c0 + GC)), u_g[k], b_ug[k], ubf_g[k], b_ubfg[k], gw, True)
                if need_h:
                    for j in range(GC):
                        kb.op(dve, lambda j=j, gw=gw, k=k: nc.vector.tensor_tensor(
                            out=gw[0][:, j, :], in0=gw[0][:, j, :], in1=hf_g[k][:, j, :], op=ALU.add),
                            reads=[b_hfg[k], gw[3][j]], writes=[gw[3][j]])
                    kb.dma(pool, S.hf[:, c0:c0 + GC, cols], gw[0][:, :, :], reads=gw[3], writes=[S.b_hf[ti]])

            ng = len(groups)
            pieces = []
            if do_dft:
                nch = S.L // 128
                Tb = S.T
                nblk = S.L // Tb
                pc = min(8, nch)
                npiece = nch // pc
                ab = kb.sb(st, "ab_all", [128, nch, D], BF16)
                b_ab = Buf()
                for ti in range(S.nt):
                    n0 = ti * S.nsub
                    kb.dma(sp, ab[:, n0:n0 + S.nsub, :],
                           S.AB[ti * S.T:(ti + 1) * S.T, :].rearrange("(s p) n -> p s n", p=128),
                           reads=[S.b_AB[ti]], writes=[b_ab])
                NTB = 3
                tabs = [kb.sb(st, "tab", [128, pc, 2, Tb], BF16) for _ in range(NTB)]
                b_tabs = [Buf() for _ in range(NTB)]
                yft = [kb.sb(st, "yft", [128, 4, Tb], BF16) for _ in range(2)]
                b_yft = [Buf(), Buf()]
                tv = [S.dft[k].rearrange("(c p) n -> p c n", p=128) for k in range(2)]
                bks = banks[0:4]

                def piece(j, q, pidx):
                    tab, b_tab = tabs[pidx % NTB], b_tabs[pidx % NTB]
                    for k in range(2):
                        kb.dma(sp, tab[:, :, k, :], tv[k][:, q * pc:(q + 1) * pc, j * Tb:(j + 1) * Tb],
                               writes=[b_tab])
                    for cc in range(pc):
                        c = q * pc + cc
                        for g in range(4):
                            mm(bks[g], bks[g][0][:, :Tb], ab[:, c, g * 256:g * 256 + 128], tab[:, cc, 0, :],
                               [b_ab, b_tab], c == 0, False)
                            mm(bks[g], bks[g][0][:, :Tb], ab[:, c, g * 256 + 128:g * 256 + 256],
                               tab[:, cc, 1, :], [b_ab, b_tab], False, c == nch - 1,
                               tick=(c == nch - 1 or (cc == pc - 1 and g == 3)))
                    if q == npiece - 1:
                        y, b_y = yft[j % 2], b_yft[j % 2]
                        for g in range(4):
                            kb.op(act, lambda g=g, y=y: nc.scalar.copy(out=y[:, g, :], in_=bks[g][0][:, :Tb]),
                                  reads=[bks[g][1]], writes=[b_y])
                        kb.dma(pool, S.yfT[:, :, j * Tb:(j + 1) * Tb], y[:, :, :], reads=[b_y],
                               writes=[S.b_yfT[j]])

                pidx = 0
                for j in range(nblk):
                    for q in range(npiece):
                        pieces.append(lambda j=j, q=q, pidx=pidx: piece(j, q, pidx))
                        pidx += 1
            bank_mode[0] = "hi"
            nslot = max(len(pieces), 1)
            per = -(-ng // nslot)
            pro_done = 0
            for gi in range(min(NB, ng)):
                g_pro(gi)
                pro_done += 1
            main_done = 0
            for sidx in range(nslot):
                if sidx % 4 == 0:
                    feed()
                tgt = min(ng, (sidx + 1) * per)
                nxt = min(ng, tgt + per)
                while main_done < tgt:
                    g_main(main_done)
                    main_done += 1
                    if pro_done < ng:
                        g_pro(pro_done)
                        pro_done += 1
                if sidx < len(pieces):
                    pieces[sidx]()
            while main_done < ng:
                g_main(main_done)
                main_done += 1
                if pro_done < ng:
                    g_pro(pro_done)
                    pro_done += 1
            bank_mode[0] = "all"
            kb.barrier()

    def sweep2b(S, l, mods, b_mods):
        T, nsub = S.T, S.nsub
        with ExitStack() as st:
            win2 = kb.sb(st, "win2", [128, 8, 3072], BF16)
            b_win2 = Buf()
            wv = w_in[l].rearrange("(c p) n -> p c n", p=128)
            for (d0, s0) in ((0, 1024), (1024, 2560), (2048, 3584)):
                kb.dma(pool, win2[:, :, d0:d0 + 1024], wv[:, :, s0:s0 + 1024], writes=[b_win2])
            wpr = kb.sb(st, "wpr", [128, 8, D], BF16)
            wpf = kb.sb(st, "wpf", [128, 4, D], BF16)
            wo = kb.sb(st, "wo", [128, 8, D], BF16)
            b_wp = Buf()
            kb.dma(pool, wpr[:], w_proj_rnn[l].rearrange("(c p) n -> p c n", p=128), writes=[b_wp])
            kb.dma(pool, wpf[:], w_proj_fourier[l].rearrange("(c p) n -> p c n", p=128), writes=[b_wp])
            kb.dma(pool, wo[:], w_out[l].rearrange("(c p) n -> p c n", p=128), writes=[b_wp])
            hxT = kb.sb(st, "hxT", [128, 8, 512], BF16)
            b_hxT = Buf()
            hch = [kb.sb(st, "hch", [128, 512], F32) for _ in range(2)]
            b_hch = [Buf(), Buf()]
            yf = kb.sb(st, "yf", [128, 4, 512], BF16)
            b_yf = Buf()
            xs = [kb.sb(st, "xs", [128, D], F32) for _ in range(2)]
            b_xs = [Buf(), Buf()]
            gg = kb.sb(st, "gg", [128, 8, 512], BF16)
            b_gg = [Buf() for _ in range(8)]
            hg = kb.sb(st, "hg", [128, 8, 512], BF16)
            b_hg = [Buf() for _ in range(8)]
            sgr = kb.sb(st, "sgr", [128, 8, 512], BF16)
            b_sgr = [Buf() for _ in range(8)]
            sgf = kb.sb(st, "sgf", [128, 8, 512], BF16)
            b_sgf = [Buf() for _ in range(8)]
            mrg = kb.sb(st, "mrg", [128, 8, 512], BF16)
            b_mrg = [Buf() for _ in range(8)]
            t1 = [kb.sb(st, "t1", [128, 512], F32) for _ in range(2)]
            b_t1 = [Buf(), Buf()]
            t2 = [kb.sb(st, "t2", [128, 512], F32) for _ in range(2)]
            b_t2 = [Buf(), Buf()]
            g1, b_g1 = mods[:, S.sidx, 2, :], b_mods[S.sidx][2]
            for ti in range(S.nt):
                feed()
                sl = slice(ti * T, (ti + 1) * T)
                kb.dma(sp, hxT[:, :, :T], S.hxT[:, :, sl], reads=[S.b_hxT[ti]], writes=[b_hxT])
                kb.dma(sp, yf[:, :, :T], S.yfT[:, :, sl], reads=[S.b_yfT[ti]], writes=[b_yf])
                for c in range(8):
                    bank = next_bank()
                    for k in range(8):
                        mm(bank, bank[0][:, :T], win2[:, k, c * 128:(c + 1) * 128], hxT[:, k, :T],
                           [b_win2, b_hxT], k == 0, k == 7)
                    kb.op(act, lambda c=c, bank=bank: nc.scalar.activation(out=gg[:, c, :T], in_=bank[0][:, :T],
                                                                          func=GELU), reads=[bank[1]],
                          writes=[b_gg[c]])
                    hc, bhc = hch[c % 2], b_hch[c % 2]
                    kb.dma(sp, hc[:, :T], S.hf[:, c, sl], reads=[S.b_hf[ti]], writes=[bhc])
                    kb.op(dve, lambda c=c, hc=hc: nc.vector.tensor_tensor(out=hg[:, c, :T], in0=gg[:, c, :T],
                                                                          in1=hc[:, :T], op=ALU.mult),
                          reads=[b_gg[c], bhc], writes=[b_hg[c]])
                for (off, dst, bd) in ((1024, sgr, b_sgr), (2048, sgf, b_sgf)):
                    for c in range(8):
                        bank = next_bank()
                        for k in range(8):
                            mm(bank, bank[0][:, :T], win2[:, k, off + c * 128:off + (c + 1) * 128], hxT[:, k, :T],
                               [b_win2, b_hxT], k == 0, k == 7)
                        kb.op(act, lambda c=c, bank=bank, dst=dst: nc.scalar.activation(
                            out=dst[:, c, :T], in_=bank[0][:, :T], func=AF.Sigmoid), reads=[bank[1]],
                            writes=[bd[c]])
                for j0 in range(0, 8, 2):
                    for j in (j0, j0 + 1):
                        bk_r = next_bank()
                        for k in range(8):
                            mm(bk_r, bk_r[0][:, :T], wpr[:, k, j * 128:(j + 1) * 128], hg[:, k, :T],
                               [b_wp, b_hg[k]], k == 0, k == 7)
                        bk_f = next_bank()
                        for g in range(4):
                            mm(bk_f, bk_f[0][:, :T], wpf[:, g, j * 128:(j + 1) * 128], yf[:, g, :T],
                               [b_wp, b_yf], g == 0, g == 3)
                        a1, ba1, a2, ba2 = t1[j % 2], b_t1[j % 2], t2[j % 2], b_t2[j % 2]
                        kb.op(dve, lambda j=j, bk=bk_r, a1=a1: nc.vector.tensor_tensor(
                            out=a1[:, :T], in0=bk[0][:, :T], in1=sgr[:, j, :T], op=ALU.mult),
                            reads=[bk_r[1], b_sgr[j]], writes=[ba1])
                        kb.op(dve, lambda j=j, bk=bk_f, a2=a2: nc.vector.tensor_tensor(
                            out=a2[:, :T], in0=bk[0][:, :T], in1=sgf[:, j, :T], op=ALU.mult),
                            reads=[bk_f[1], b_sgf[j]], writes=[ba2])
                    for j in (j0, j0 + 1):
                        a1, ba1, a2, ba2 = t1[j % 2], b_t1[j % 2], t2[j % 2], b_t2[j % 2]
                        kb.op(dve, lambda j=j, a1=a1, a2=a2: nc.vector.tensor_tensor(
                            out=mrg[:, j, :T], in0=a1[:, :T], in1=a2[:, :T], op=ALU.add),
                            reads=[ba1, ba2], writes=[b_mrg[j]])
                for s in range(nsub):
                    x1, bx1 = xs[s % 2], b_xs[s % 2]
                    r0 = ti * T + s * 128
                    srcx = S.src if S.first else S.xres
                    kb.dma(sp, x1[:], srcx[r0:r0 + 128, :], reads=([] if S.first else [S.b_xres[ti]]), writes=[bx1])
                    for dh in range(2):
                        bank = next_bank()
                        for k in range(8):
                            mm(bank, bank[0][:, :], mrg[:, k, s * 128:(s + 1) * 128], wo[:, k, dh * 512:(dh + 1) * 512],
                               [b_wp, b_mrg[k]], k == 0, k == 7)
                        a1, ba1 = t1[dh], b_t1[dh]
                        kb.op(dve, lambda dh=dh, bank=bank, a1=a1: nc.vector.tensor_tensor(
                            out=a1[:, :], in0=bank[0][:, :], in1=g1[:, dh * 512:(dh + 1) * 512], op=ALU.mult),
                            reads=[bank[1], b_g1], writes=[ba1])
                    for dh in range(2):
                        a1, ba1 = t1[dh], b_t1[dh]
                        kb.op(dve, lambda dh=dh, a1=a1, x1=x1: nc.vector.tensor_tensor(
                            out=x1[:, dh * 512:(dh + 1) * 512], in0=x1[:, dh * 512:(dh + 1) * 512],
                            in1=a1[:, :], op=ALU.add), reads=[ba1, bx1], writes=[bx1])
                    kb.dma(pool, S.xres[r0:r0 + 128, :], x1[:], reads=[bx1], writes=[S.b_xres[ti]])
            S.first = False
            kb.barrier()

    def moe_pre(l, streams, mods, b_mods):
        tiles = []
        NS = sum(S.L // 128 for S in streams)
        NTILE = (2 * NS * 128 + NE * 511) // 512
        assert NTILE <= 32 and NS <= NSMAX
        with ExitStack() as st:
            wkn = norm_work(st)
            hTf = kb.sb(st, "hTf", [128, 8, 512], F32)
            b_hTf = [Buf() for _ in range(8)]
            h2tok = kb.sb(st, "h2tok", [128, NS, D], BF16)
            b_h2tok = [Buf() for _ in range(NS)]
            lg = kb.sb(st, "lg", [128, NS, NE], F32)
            b_lg = Buf()
            m0, sub0 = 0, 0
            for S in streams:
                G, bG, Sh, bSh = mods[:, S.sidx, 1, :], b_mods[S.sidx][1], mods[:, S.sidx, 0, :], b_mods[S.sidx][0]
                for ti in range(S.nt):
                    T = S.T
                    norm_transpose_tile(S, ti, wkn, G, bG, Sh, bSh, [(dve, hTf, b_hTf)],
                                        tok_out=(h2tok, b_h2tok, sub0))
                    subs = []
                    for s in range(S.nsub):
                        bank = next_bank()
                        for k in range(8):
                            mm(bank, bank[0][:, :NE], hTf[:, k, s * 128:(s + 1) * 128], rw[:, k, :NE],
                               [b_hTf[k], b_rw], k == 0, k == 7)
                        kb.op(dve, lambda bank=bank, si=sub0 + s: nc.vector.tensor_copy(out=lg[:, si, :],
                                                                                      in_=bank[0][:, :NE]),
                              reads=[bank[1]], writes=[b_lg])
                        subs.append(sub0 + s)
                    tiles.append(dict(S=S, ti=ti, m0=m0, T=T, subs=subs))
                    m0 += T
                    sub0 += S.nsub
            mk3 = lambda nm: kb.sb(st, nm, [128, NS, NE], F32)
            aff, sel, sel2, Mk, gts, Cn, Wn, off, posf, hiM, tmp = [mk3(n_) for n_ in (
                "aff", "sel", "sel2", "Mk", "gts", "Cn", "Wn", "off", "posf", "hiM", "tmp")]
            m1 = kb.sb(st, "m1", [128, NS * 4], F32)
            m2 = kb.sb(st, "m2", [128, NS * 4], F32)
            gs = kb.sb(st, "gs", [128, NS * 4], F32)
            gmax = kb.sb(st, "gmax", [128, NS], F32)
            ws = kb.sb(st, "ws", [128, NS], F32)
            phi = kb.sb(st, "phi", [128, NS], F32)
            plo = kb.sb(st, "plo", [128, NS], F32)
            pos2f = kb.sb(st, "pos2f", [128, NS, 2], F32)
            tot = kb.sb(st, "tot", [128, NE], F32)
            ntl = kb.sb(st, "ntl", [128, NE], F32)
            baseT = kb.sb(st, "baseT", [128, NE], F32)
            endT = kb.sb(st, "endT", [128, NE], F32)
            base = kb.sb(st, "base", [128, NE], F32)
            cmp = kb.sb(st, "cmp", [128, NE, 16], F32)
            cmp2 = kb.sb(st, "cmp2", [128, 32, NE], F32)
            ef = kb.sb(st, "ef", [128, 32], F32)
            b_r = Buf()
            rd = [b_lg, b_r, b_rb, b_rc]
            v = lambda t: t[:].rearrange("p n e -> p (n e)")
            v4 = lambda t: t[:].rearrange("p n (g e) -> p (n g) e", e=4)
            g3 = lambda t: t[:].rearrange("p (n g) -> p n g", g=4)
            R = lambda fn: kb.op(dve, fn, reads=rd, writes=[b_r])
            TT = lambda o, a, b_, op: R(lambda: nc.vector.tensor_tensor(out=o, in0=a, in1=b_, op=op))
            RED = lambda o, a, op: R(lambda: nc.vector.tensor_reduce(out=o, in_=a, axis=AX.X, op=op))
            kb.op(act, lambda: nc.scalar.activation(out=v(aff), in_=v(lg), func=AF.Sigmoid), reads=rd, writes=[b_r])
            TT(sel[:], aff[:], bc_mid(rb_bc[:], NS), ALU.add)
            RED(m1[:], v4(sel), ALU.max)
            TT(v4(sel2), v4(sel), bc_last(m1[:], 4), ALU.is_equal)
            R(lambda: nc.vector.scalar_tensor_tensor(out=v(sel2), in0=v(sel2), scalar=-BIG, in1=v(sel),
                                                     op0=ALU.mult, op1=ALU.add))
            RED(m2[:], v4(sel2), ALU.max)
            TT(gs[:], m1[:], m2[:], ALU.add)
            RED(gmax[:], g3(gs), ALU.max)
            TT(g3(gs), g3(gs), bc_last(gmax[:], 4), ALU.is_equal)
            TT(v4(Mk), v4(sel), bc_last(m2[:], 4), ALU.is_ge)
            TT(v4(Mk), v4(Mk), bc_last(gs[:], 4), ALU.mult)
            TT(v(sel2), v(Mk), v(aff), ALU.mult)
            RED(ws[:], sel2[:], ALU.add)
            R(lambda: nc.vector.reciprocal(out=ws[:], in_=ws[:]))
            TT(gts[:], sel2[:], bc_last(ws[:], NE), ALU.mult)
            for n0 in range(0, NS, 32):
                n1 = min(NS, n0 + 32)
                w_ = (n1 - n0) * NE
                mv = Mk[:, n0:n1, :].rearrange("p n e -> p (n e)")
                bk = next_bank()
                mm(bk, bk[0][:, :w_], ones[:], mv, [b_ones, b_r], True, True)
                kb.op(dve, lambda bk=bk, n0=n0, n1=n1, w_=w_: nc.vector.tensor_copy(
                    out=Cn[:, n0:n1, :].rearrange("p n e -> p (n e)"), in_=bk[0][:, :w_]),
                    reads=[bk[1]] + rd, writes=[b_r])
                bk2 = next_bank()
                mm(bk2, bk2[0][:, :w_], rc[:, 0:128], mv, [b_rc, b_r], True, True)
                kb.op(dve, lambda bk2=bk2, n0=n0, n1=n1, w_=w_: nc.vector.tensor_copy(
                    out=Wn[:, n0:n1, :].rearrange("p n e -> p (n e)"), in_=bk2[0][:, :w_]),
                    reads=[bk2[1]] + rd, writes=[b_r])
            R(lambda: nc.vector.memset(off[:, 0, :], 0.0))
            for n in range(1, NS):
                TT(off[:, n, :], off[:, n - 1, :], Cn[:, n - 1, :], ALU.add)
            TT(tot[:], off[:, NS - 1, :], Cn[:, NS - 1, :], ALU.add)
            TT(cmp[:], bc_last(tot[:], 16), bc_mid(rc[:, 128:144], NE), ALU.is_gt)
            RED(ntl[:], cmp[:], ALU.add)
            TT(cmp[:], bc_mid(ntl[:], NE), rc[:, 144:400].rearrange("p (e f) -> p e f", f=16), ALU.mult)
            RED(baseT[:], cmp[:], ALU.add)
            TT(endT[:], baseT[:], ntl[:], ALU.add)
            R(lambda: nc.vector.tensor_scalar(out=base[:], in0=baseT[:], scalar1=512.0, scalar2=None, op0=ALU.mult))
            TT(cmp2[:, :NTILE, :], bc_last(rc[:, 400:400 + NTILE], NE), bc_mid(endT[:], NTILE), ALU.is_ge)
            RED(ef[:, :NTILE], cmp2[:, :NTILE, :], ALU.add)
            R(lambda: nc.vector.tensor_scalar(out=ef[:, :NTILE], in0=ef[:, :NTILE], scalar1=float(NE - 1),
                                              scalar2=128.0, op0=ALU.min, op1=ALU.mult))
            R(lambda: nc.vector.tensor_scalar(out=ef[:, :NTILE], in0=ef[:, :NTILE], scalar1=rc[:, 432:433],
                                              scalar2=None, op0=ALU.add))
            kb.op(dve, lambda: nc.vector.tensor_copy(out=widx[:, :NTILE], in_=ef[:, :NTILE]), reads=rd,
                  writes=[b_route])
            TT(v(posf), v(Wn), v(off), ALU.add)
            TT(posf[:], posf[:], bc_mid(base[:], NS), ALU.add)
            R(lambda: nc.vector.scalar_tensor_tensor(out=v(posf), in0=v(posf), scalar=1.0, in1=v(Mk),
                                                     op0=ALU.add, op1=ALU.mult))
            RED(phi[:], posf[:], ALU.max)
            TT(hiM[:], posf[:], bc_last(phi[:], NE), ALU.is_equal)
            TT(v(tmp), v(hiM), v(gts), ALU.mult)
            kb.op(dve, lambda: nc.vector.tensor_reduce(out=gate2[:, :NS, 1], in_=tmp[:], axis=AX.X, op=ALU.add),
                  reads=rd, writes=[b_route])
            TT(v(hiM), v(Mk), v(hiM), ALU.subtract)
            TT(v(tmp), v(hiM), v(gts), ALU.mult)
            kb.op(dve, lambda: nc.vector.tensor_reduce(out=gate2[:, :NS, 0], in_=tmp[:], axis=AX.X, op=ALU.add),
                  reads=rd, writes=[b_route])
            TT(v(tmp), v(hiM), v(posf), ALU.mult)
            RED(plo[:], tmp[:], ALU.add)
            R(lambda: nc.vector.tensor_scalar(out=pos2f[:, :, 0], in0=plo[:], scalar1=-1.0,
                                              scalar2=float(NSLOT - 1), op0=ALU.add, op1=ALU.min))
            R(lambda: nc.vector.tensor_scalar(out=pos2f[:, :, 1], in0=phi[:], scalar1=-1.0,
                                              scalar2=float(NSLOT - 1), op0=ALU.add, op1=ALU.min))
            R(lambda: nc.vector.tensor_scalar(out=pos2f[:].rearrange("p n k -> p (n k)"),
                                              in0=pos2f[:].rearrange("p n k -> p (n k)"), scalar1=0.0,
                                              scalar2=None, op0=ALU.max))
            kb.op(dve, lambda: nc.vector.tensor_copy(out=pos2i[:, :NS, :].rearrange("p n k -> p (n k)"),
                                                     in_=pos2f[:].rearrange("p n k -> p (n k)")),
                  reads=rd, writes=[b_route])
            if kb.dump:
                gd = kb.dram("gates_l%d" % l, [128, NS, NE], F32)
                kb.dma(sp, gd, gts[:], reads=[b_r])
                pd = kb.dram("pos_l%d" % l, [128, NS, 2], F32)
                kb.dma(sp, pd, pos2f[:], reads=[b_r])
            for n in range(NS):
                for k in range(2):
                    kb.idma(h2s_d[:, :], IOA(ap=pos2i[:, n, k:k + 1], axis=0), h2tok[:, n, :], None,
                            reads=[b_route, b_h2tok[n]])
            kb.barrier()
        return tiles, NTILE

    def moe_experts(l, tiles, NTILE, last):
        wv = wbf_d[l]
        with ExitStack() as st:
            NSL = 6
            wsl = [kb.sb(st, "wsl", [128, 8, D], BF16) for _ in range(NSL)]
            b_wsl = [Buf() for _ in range(NSL)]
            ht = [kb.sb(st, "ht", [128, 4, D], BF16) for _ in range(2)]
            b_ht = [Buf(), Buf()]
            hT = [kb.sb(st, "hT", [128, 8, 512], BF16) for _ in range(2)]
            b_hT = [[Buf() for _ in range(8)] for _ in range(2)]
            he = [kb.sb(st, "he", [128, 8, 512], BF16) for _ in range(2)]
            b_he = [[Buf() for _ in range(8)] for _ in range(2)]
            stmp = [kb.sb(st, "stmp", [128, 512], BF16) for _ in range(2)]
            b_stmp = [Buf(), Buf()]
            ystage = kb.sb(st, "ystage", [128, 4, D], F32)
            b_ys = [Buf() for _ in range(4)]

            def load_w(j, m):
                if j >= NTILE:
                    return
                si = 3 * (j % 2) + m
                kb.idma(wsl[si][:].rearrange("p c f -> p (c f)"), None, wv[m], IOA(ap=widx[:, j:j + 1], axis=0),
                        reads=[b_route], writes=[b_wsl[si]])

            def load_ht(j):
                if j >= NTILE:
                    return
                kb.dma(sp, ht[j % 2][:], h2s_d[j * 512:(j + 1) * 512, :].rearrange("(s p) d -> p s d", p=128),
                       writes=[b_ht[j % 2]])

            def emit_H(j):
                T = 512
                s1, s3 = 3 * (j % 2), 3 * (j % 2) + 1
                htj, bht = ht[j % 2], b_ht[j % 2]
                hb_, bhb = hT[j % 2], b_hT[j % 2]
                for c in range(8):
                    bk = next_bank()
                    for s in range(4):
                        mm(bk, bk[0][:, s * 128:(s + 1) * 128], htj[:, s, c * 128:(c + 1) * 128], identb[:],
                           [bht, b_identb], True, True, tick=(s == 3))
                    if c % 2 == 0:
                        kb.op(act, lambda bk=bk, c=c, hb_=hb_: nc.scalar.copy(out=hb_[:, c, :], in_=bk[0][:, :]),
                              reads=[bk[1]], writes=[bhb[c]])
                    else:
                        kb.op(dve, lambda bk=bk, c=c, hb_=hb_: nc.vector.tensor_copy(out=hb_[:, c, :],
                                                                                   in_=bk[0][:, :]),
                              reads=[bk[1]], writes=[bhb[c]])
                load_ht(j + 2)
                hh, bhh = he[j % 2], b_he[j % 2]
                for fc in range(8):
                    bk1 = next_bank()
                    for k in range(8):
                        mm(bk1, bk1[0][:, :T], wsl[s1][:, k, fc * 128:(fc + 1) * 128], hb_[:, k, :T],
                           [b_wsl[s1], bhb[k]], k == 0, k == 7)
                    bk3 = next_bank()
                    for k in range(8):
                        mm(bk3, bk3[0][:, :T], wsl[s3][:, k, fc * 128:(fc + 1) * 128], hb_[:, k, :T],
                           [b_wsl[s3], bhb[k]], k == 0, k == 7)
                    sm, bsm = stmp[fc % 2], b_stmp[fc % 2]
                    kb.op(act, lambda bk1=bk1, sm=sm, T=T: nc.scalar.activation(out=sm[:, :T], in_=bk1[0][:, :T],
                                                                              func=AF.Silu),
                          reads=[bk1[1]], writes=[bsm])
                    kb.op(dve, lambda fc=fc, bk3=bk3, sm=sm, hh=hh, T=T: nc.vector.tensor_tensor(
                        out=hh[:, fc, :T], in0=bk3[0][:, :T], in1=sm[:, :T], op=ALU.mult),
                        reads=[bk3[1], bsm], writes=[bhh[fc]])

            def emit_Y(j):
                s2 = 3 * (j % 2) + 2
                hh, bhh = he[j % 2], b_he[j % 2]
                for s in range(4):
                    for dh in range(2):
                        bank = next_bank()
                        for fc in range(8):
                            mm(bank, bank[0][:, :], hh[:, fc, s * 128:(s + 1) * 128],
                               wsl[s2][:, fc, dh * 512:(dh + 1) * 512], [bhh[fc], b_wsl[s2]], fc == 0, fc == 7)
                        ya = ystage[:, s, dh * 512:(dh + 1) * 512]
                        if dh == 0:
                            kb.op(act, lambda bank=bank, ya=ya: nc.scalar.copy(out=ya, in_=bank[0][:, :]),
                                  reads=[bank[1]], writes=[b_ys[s]])
                        else:
                            kb.op(dve, lambda bank=bank, ya=ya: nc.vector.tensor_copy(out=ya, in_=bank[0][:, :]),
                                  reads=[bank[1]], writes=[b_ys[s]])
                    r0 = j * 512 + s * 128
                    kb.dma(sp, ys_d[r0:r0 + 128, :], ystage[:, s, :], reads=[b_ys[s]])

            for j in range(2):
                for m in range(3):
                    load_w(j, m)
            load_ht(0)
            load_ht(1)
            emit_H(0)
            load_w(2, 0)
            load_w(2, 1)
            for j in range(NTILE):
                if j + 1 < NTILE:
                    emit_H(j + 1)
                    load_w(j + 3, 0)
                    load_w(j + 3, 1)
                emit_Y(j)
                load_w(j + 2, 2)
            kb.barrier()
        with ExitStack() as st:
            ND = 4
            ga = [[kb.sb(st, "ga", [128, D], F32) for _ in range(2)] for _ in range(ND)]
            b_ga = [[Buf(), Buf()] for _ in range(ND)]
            xs = [kb.sb(st, "xs", [128, D], F32) for _ in range(ND)]
            b_xs = [Buf() for _ in range(ND)]
            sq = kb.sb(st, "sq", [128, D], F32)
            b_sq = Buf()
            ss = kb.sb(st, "ss2", [128, ND], F32)
            b_ss = [Buf() for _ in range(ND)]
            n = 0
            for t in tiles:
                S = t["S"]
                for s, sg in enumerate(t["subs"]):
                    x1, bx1 = xs[n % ND], b_xs[n % ND]
                    g_, bg_ = ga[n % ND], b_ga[n % ND]
                    n += 1
                    r0 = t["ti"] * S.T + s * 128
                    for k in range(2):
                        kb.idma(g_[k][:], None, ys_d[:, :], IOA(ap=pos2i[:, sg, k:k + 1], axis=0),
                                reads=[b_route], writes=[bg_[k]])
                    kb.dma(sp, x1[:], S.xres[r0:r0 + 128, :], reads=[S.b_xres[t["ti"]]], writes=[bx1])
                    kb.op(act, lambda g_=g_, sg=sg: nc.scalar.mul(out=g_[0][:], in_=g_[0][:], mul=gate2[:, sg, 0:1]),
                          reads=[bg_[0], b_route], writes=[bg_[0]])
                    kb.op(dve, lambda g_=g_, sg=sg: nc.vector.scalar_tensor_tensor(
                        out=g_[0][:], in0=g_[1][:], scalar=gate2[:, sg, 1:2], in1=g_[0][:], op0=ALU.mult,
                        op1=ALU.add), reads=[bg_[0], bg_[1], b_route], writes=[bg_[0]])
                    kb.op(dve, lambda g_=g_, S=S: nc.vector.tensor_tensor(out=g_[0][:], in0=g_[0][:],
                                                                         in1=g2[:, S.sidx, :], op=ALU.mult),
                          reads=[bg_[0], b_g2[S.sidx]], writes=[bg_[0]])
                    kb.op(dve, lambda x1=x1, g_=g_: nc.vector.tensor_tensor(out=x1[:], in0=x1[:], in1=g_[0][:],
                                                                         op=ALU.add),
                          reads=[bg_[0], bx1], writes=[bx1])
                    if not last:
                        kb.dma(sp, S.xres[r0:r0 + 128, :], x1[:], reads=[bx1], writes=[S.b_xres[t["ti"]]])
                    else:
                        sc, bsc = ss[:, (n % ND):(n % ND) + 1], b_ss[n % ND]
                        kb.op(act, lambda x1=x1, sc=sc: nc.scalar.activation(
                            out=sq[:], in_=x1[:], func=AF.Square, accum_out=sc),
                            reads=[bx1], writes=[b_sq, bsc])
                        kb.op(dve, lambda sc=sc: nc.vector.tensor_scalar(out=sc, in0=sc, scalar1=1.0 / D,
                                                                        scalar2=EPS, op0=ALU.mult, op1=ALU.add),
                              reads=[bsc], writes=[bsc])
                        kb.op(act, lambda sc=sc: nc.scalar.activation(out=sc, in_=sc, func=AF.Ln),
                              reads=[bsc], writes=[bsc])
                        kb.op(act, lambda sc=sc: nc.scalar.activation(out=sc, in_=sc, func=AF.Exp, scale=-0.5),
                              reads=[bsc], writes=[bsc])
                        kb.op(dve, lambda x1=x1, sc=sc: nc.vector.scalar_tensor_tensor(
                            out=x1[:], in0=x1[:], scalar=sc, in1=gF_bc[:], op0=ALU.mult, op1=ALU.mult),
                            reads=[bx1, bsc, b_gF], writes=[bx1])
                        tk = kb.dma(sp, out_d[r0:r0 + 128, :], x1[:], reads=[bx1])
                        out_toks.append(tk)
            kb.barrier()

    out_toks = []
    GELU = AF.Gelu_apprx_tanh

    def stop(tag):
        return kb.stop_after == tag

    done = False
    for l in range(DEPTH):
        last = l == DEPTH - 1
        with ExitStack() as lst:
            mods = kb.sb(lst, "mods", [128, 2, 3, D], F32)
            b_mods = [[Buf() for _ in range(3)] for _ in range(2)]
            cast_l[0] = l
            mod_phase(l, 0, mods, b_mods, norm_mix_g)
            if l == 0:
                for r0 in range(0, NSLOT, 1024):
                    kb.dma(pool, h2s_d[r0:r0 + 1024, :].rearrange("(p c) d -> p c d", c=8), bc_mid(zrow[:], 8),
                           reads=[b_zrow])
            for d in range(2):
                kb.op(dve, lambda d=d: nc.vector.memset(carry[:, d, :], 0.0), writes=b_carry[d])
            sweep1(SC, l, mods, b_mods, do_four=not last)
            sweep2a_dft(SC, l, need_h=not last, do_dft=not last)
            if stop("ctx_s2a_l%d" % l) or stop("ctx_dft_l%d" % l):
                done = True
                break
            if not last:
                sweep2b(SC, l, mods, b_mods)
                if stop("ctx_mixer_l%d" % l):
                    done = True
                    break
            sweep1(SX, l, mods, b_mods, do_four=True)
            if stop("x_s1_l%d" % l):
                done = True
                break
            sweep2a_dft(SX, l, need_h=True, do_dft=True)
            if stop("x_dft_l%d" % l):
                done = True
                break
            sweep2b(SX, l, mods, b_mods)
            if stop("mixer_l%d" % l):
                done = True
                break
            feed(3 * NE)
            mod_phase(l, 1, mods, b_mods, norm_ffn_g)
            for s in range(2):
                kb.op(dve, lambda s=s: nc.vector.tensor_copy(out=g2[:, s, :], in_=mods[:, s, 2, :]),
                      reads=[b_mods[s][2]], writes=[b_g2[s]])
            tiles, ntile = moe_pre(l, [SX] if last else [SC, SX], mods, b_mods)
            if stop("moe_pre_l%d" % l):
                done = True
                break
        moe_experts(l, tiles, ntile, last)
        if stop("layer_%d" % l):
            done = True
            break

    kb.barrier()
    for tk in out_toks:
        sp.wait(tk)
    kb.es.close()
    return nc


def _consts():
    c = np.arange(128)
    ang = 2.0 * np.pi * np.outer(c, c) / 128.0
    csc = np.concatenate([np.cos(ang), np.sin(ang)], axis=1) / np.sqrt(128.0)

    def tab(n):
        t = np.arange(n, dtype=np.int64)
        m = (np.outer(t, t) % n).astype(np.float64)
        a = 2.0 * np.pi * m / n
        s = 1.0 / np.sqrt(float(n))
        return np.stack([np.cos(a) * s, -np.sin(a) * s]).astype(np.float32).astype(ml_dtypes.bfloat16)

    rcst = np.zeros((128, 448), np.float32)
    rcst[:, 0:128] = (c[:, None] < c[None, :])
    rcst[:, 128:144] = 512.0 * np.arange(16)[None, :]
    e16 = np.arange(16)
    rcst[:, 144:400] = (e16[None, :] < e16[:, None]).astype(np.float32).reshape(1, 256)
    rcst[:, 400:432] = np.arange(32)[None, :]
    rcst[:, 432] = c
    return (np.eye(128, dtype=np.float32), csc.astype(np.float32), tab(L), tab(LC), rcst)


_CACHE = {}


def make_in_maps(inputs):
    if "consts" not in _CACHE:
        _CACHE["consts"] = _consts()
    ident, csc, dftl, dftc, rcst = _CACHE["consts"]
    f = lambda a: np.ascontiguousarray(np.asarray(a, dtype=np.float32))
    shared = {k: f(inputs[k]) for k in (
        "ada_w", "ada_b", "norm_mix_g", "w_in", "conv_w", "conv_b", "lru_w_a", "lru_b_a", "lru_w_x", "lru_b_x",
        "lru_lambda", "w_proj_rnn", "w_proj_fourier", "w_out", "norm_ffn_g", "router_w")}
    for k in ("moe_w1", "moe_w3", "moe_w2"):
        w = f(inputs[k]).reshape(DEPTH, NE, 8, 128, D)
        shared[k] = np.ascontiguousarray(w.transpose(0, 1, 3, 2, 4)).reshape(DEPTH, NE, D, D)
    shared["router_b"] = f(inputs["router_b"]).reshape(1, NE)
    shared["final_norm_g"] = f(inputs["final_norm_g"]).reshape(1, D)
    shared.update(ident=ident, csc=csc, dftl=dftl, dftc=dftc, rconst=rcst)
    x, c, ctx, c_ctx = f(inputs["x"]), f(inputs["c"]), f(inputs["ctx"]), f(inputs["c_ctx"])
    maps = []
    for b in range(8):
        m = dict(shared)
        m["x"] = x[b]
        m["ctx"] = ctx[b]
        m["cc"] = np.ascontiguousarray(np.stack([c[b], c_ctx]))
        maps.append(m)
    return maps


def kernel(**inputs):
    if "nc" not in _CACHE:
        _CACHE["nc"] = build_program()
    nc = _CACHE["nc"]
    maps = make_in_maps(inputs)
    res = run_bass_kernel_spmd(nc, maps, core_ids=list(range(8)))
    return np.stack([np.asarray(r["out"], dtype=np.float32) for r in res.results], axis=0)
```

```python
import numpy as np
import ml_dtypes
from contextlib import ExitStack
import concourse.bass as bass
import concourse.mybir as mybir
from concourse.bass_utils import run_bass_kernel_spmd

F32 = mybir.dt.float32
BF16 = mybir.dt.bfloat16
I32 = mybir.dt.int32
IOA = bass.IndirectOffsetOnAxis
NSLOT = 32 * 512
AF = mybir.ActivationFunctionType
ALU = mybir.AluOpType
AX = mybir.AxisListType

D = 1024
L = 4096
LC = 256
DEPTH = 2
NE = 16
EPS = 1e-6
SEM_LIMIT = 500
BIG = 1.0e9


class Buf:
    __slots__ = ("name", "w", "r", "psum")

    def __init__(self, name="", psum=False):
        self.name = name
        self.w = None
        self.r = {}
        self.psum = psum


class EngQ:
    def __init__(self, kb, eng, name, is_pe=False):
        self.kb = kb
        self.eng = eng
        self.name = name
        self.is_pe = is_pe
        self.cur = None
        self.cnt = 0
        self.waited = {}
        self.last = None

    def wait(self, tok):
        if tok is None or tok[3] != self.kb.epoch:
            return
        sem, c, key = tok[0], tok[1], tok[2]
        if self.waited.get(key, 0) >= c:
            return
        self.eng.wait_ge(sem, c)
        self.waited[key] = c

    def tick(self, inst):
        if self.cur is None or self.cnt >= SEM_LIMIT:
            self.cur = self.kb.new_sem(self.name)
            self.cnt = 0
        self.cnt += 1
        inst.then_inc(self.cur[0], 1)
        if self.kb.rec is not None:
            self.kb.rec_inc(self, self.cur[0], self.cur[1], 1, self.cnt - 1)
        self.last = (self.cur[0], self.cnt, self.cur[1], self.kb.epoch)
        return self.last


class KB:
    def __init__(self, dump=False, stop_after=None):
        self.nc = bass.Bass("TRN2", target_bir_lowering=False)
        self.dump = dump
        self.stop_after = stop_after
        self.es = ExitStack()
        self.nsem = 0
        nc = self.nc
        self.pe = EngQ(self, nc.tensor, "pe", is_pe=True)
        self.act = EngQ(self, nc.scalar, "act")
        self.dve = EngQ(self, nc.vector, "dve")
        self.pool = EngQ(self, nc.gpsimd, "pool")
        self.sp = EngQ(self, nc.sync, "sp")
        self.engs = [self.pe, self.act, self.dve, self.pool, self.sp]
        self.dma_sems = []
        self.dma_rr = 0
        self.dma_live = {}
        self.uid = 0
        self.pe_pending = {}
        self.epoch = 0
        self.my_sems = []
        self.rec = None

    def new_sem(self, name):
        self.nsem += 1
        s = self.nc.alloc_semaphore(name="s%s%d" % (name, self.nsem))
        self.my_sems.append(s)
        return (s, self.nsem)

    def dma_sem(self):
        if len(self.dma_sems) < 40:
            s, key = self.new_sem("dma")
            self.dma_sems.append([s, key, 0])
            ent = self.dma_sems[-1]
        else:
            i = self.dma_rr % 40
            if self.dma_sems[i][2] >= 30 * 16:
                s, key = self.new_sem("dma")
                self.dma_sems[i] = [s, key, 0]
            ent = self.dma_sems[i]
        self.dma_rr += 1
        return ent

    def op(self, q, fn, reads=(), writes=(), tick=True):
        for b in reads:
            q.wait(b.w)
            if b.psum:
                for t in b.r.values():
                    q.wait(t)
        for b in writes:
            assert q.is_pe or id(b) not in self.pe_pending, "write to buffer with un-ticked PE reads: %s" % b.name
            if not q.is_pe:
                q.wait(b.w)
            elif b.w is not None and not self._is_pe_tok(b.w):
                q.wait(b.w)
            for t in b.r.values():
                q.wait(t)
        inst = fn()
        if not tick:
            assert q.is_pe
            for b in reads:
                self.pe_pending[id(b)] = b
            for b in writes:
                b.r = {}
            return None
        tok = q.tick(inst)
        if q.is_pe and self.pe_pending:
            for b in self.pe_pending.values():
                b.r[tok[2]] = tok
            self.pe_pending = {}
        for b in reads:
            b.r[tok[2]] = tok
        for b in writes:
            b.w = tok
            b.r = {}
        return tok

    def _is_pe_tok(self, tok):
        return tok[2] in self.pe_keys

    @property
    def pe_keys(self):
        if not hasattr(self, "_pe_keys"):
            self._pe_keys = set()
        return self._pe_keys

    def pe_op(self, fn, reads=(), writes=(), tick=True):
        tok = self.op(self.pe, fn, reads, writes, tick=tick)
        if tok is not None:
            self.pe_keys.add(tok[2])
        return tok

    def dma(self, q, out, in_, reads=(), writes=(), **kw):
        ent = self.dma_sem()
        sem, key, tot = ent
        if tot > 0:
            q.wait((sem, tot, key, self.epoch))
        for b in reads:
            q.wait(b.w)
        for b in writes:
            assert id(b) not in self.pe_pending, "dma write to buffer with un-ticked PE reads: %s" % b.name
            q.wait(b.w)
            for t in b.r.values():
                q.wait(t)
        q.eng.dma_start(out=out, in_=in_, **kw).then_inc(sem, 16)
        if self.rec is not None:
            self.rec_inc(q, sem, key, 16, tot)
        ent[2] = tot + 16
        tok = (sem, tot + 16, key, self.epoch)
        self.dma_live[key] = tok
        for b in reads:
            b.r[key] = tok
        for b in writes:
            b.w = tok
            b.r = {}
        return tok

    def idma(self, out, out_off, in_, in_off, reads=(), writes=()):
        q = self.pool
        ent = self.dma_sem()
        sem, key, tot = ent
        if tot > 0:
            q.wait((sem, tot, key, self.epoch))
        for b in reads:
            q.wait(b.w)
        for b in writes:
            assert id(b) not in self.pe_pending, "dma write to buffer with un-ticked PE reads: %s" % b.name
            q.wait(b.w)
            for t in b.r.values():
                q.wait(t)
        self.nc.gpsimd.indirect_dma_start(out=out, out_offset=out_off, in_=in_, in_offset=in_off).then_inc(sem, 16)
        ent[2] = tot + 16
        tok = (sem, tot + 16, key, self.epoch)
        self.dma_live[key] = tok
        for b in reads:
            b.r[key] = tok
        for b in writes:
            b.w = tok
            b.r = {}
        return tok

    def rec_inc(self, q, sem, key, amount, before):
        d = self.rec.setdefault(q.name, {})
        if key not in d:
            d[key] = [sem, 0, before]
        d[key][1] += amount

    def cond_block(self, cnt_regs, thr, body, engines=None):
        engines = engines or self.engs
        assert self.rec is None and not self.pe_pending
        self.rec = {}
        with self.nc.If(cnt_regs > thr):
            body()
            assert not self.pe_pending
        rec, self.rec = self.rec, None
        with self.nc.Else():
            for q in self.engs:
                incs = rec.get(q.name, {})
                for key, (sem, amount, before) in incs.items():
                    if before > 0:
                        q.eng.wait_ge(sem, before)
                    left = amount
                    while left > 0:
                        a = min(left, 16)
                        q.eng.nop().then_inc(sem, a)
                        left -= a
        for q in self.engs:
            q.waited = {}

    def load_count_regs(self, ap, engines=None):
        return self.nc.values_load(ap, min_val=0, max_val=16384)

    def barrier(self):
        toks = [q.last for q in self.engs if q.last is not None] + list(self.dma_live.values())
        for q in self.engs:
            for t in toks:
                q.wait(t)
        assert not self.pe_pending
        self.nc.all_engine_barrier()
        self.nc.clear_and_free_semaphores(list(self.my_sems))
        self.nc.all_engine_barrier()
        self.my_sems = []
        self.epoch += 1
        for q in self.engs:
            q.cur, q.cnt, q.waited, q.last = None, 0, {}, None
        self.dma_sems, self.dma_rr, self.dma_live = [], 0, {}
        self._pe_keys = set()

    def sb(self, st, name, shape, dtype):
        self.uid += 1
        return st.enter_context(self.nc.sbuf_tensor("%s_%d" % (name, self.uid), list(shape), dtype))

    def dram(self, name, shape, dtype, dumpable=True):
        kind = "ExternalOutput" if (self.dump and dumpable) else "Internal"
        return self.nc.dram_tensor(name, list(shape), dtype, kind=kind).ap()


def bc_last(ap, n):
    pat = [list(x) for x in ap.ap] + [[0, n]]
    return bass.AP(ap.tensor, ap.offset, pat)


def bc_mid(ap, n):
    pat = [list(x) for x in ap.ap]
    return bass.AP(ap.tensor, ap.offset, [pat[0], [0, n]] + pat[1:])


def rev_last(ap):
    pat = [list(x) for x in ap.ap]
    step, n = pat[-1]
    pat[-1] = [-step, n]
    return bass.AP(ap.tensor, ap.offset + step * (n - 1), pat)


class Stream:
    pass


def build_program(dump=False, stop_after=None):
    kb = KB(dump=dump, stop_after=stop_after)
    nc = kb.nc
    pe, act, dve, pool, sp = kb.pe, kb.act, kb.dve, kb.pool, kb.sp

    def din(name, shape, dtype=F32):
        return nc.dram_tensor(name, list(shape), dtype, kind="ExternalInput").ap()

    x_in = din("x", [L, D])
    ctx_in = din("ctx", [LC, D])
    cc_in = din("cc", [2, D])
    ada_w = din("ada_w", [DEPTH, D, 6 * D])
    ada_b = din("ada_b", [DEPTH, 6 * D])
    norm_mix_g = din("norm_mix_g", [DEPTH, D])
    w_in = din("w_in", [DEPTH, D, 4608])
    conv_w = din("conv_w", [DEPTH, 4, D])
    conv_b = din("conv_b", [DEPTH, D])
    lru_w_a = din("lru_w_a", [DEPTH, 2, 8, 128, 128])
    lru_b_a = din("lru_b_a", [DEPTH, 2, 8, 128])
    lru_w_x = din("lru_w_x", [DEPTH, 2, 8, 128, 128])
    lru_b_x = din("lru_b_x", [DEPTH, 2, 8, 128])
    lru_lambda = din("lru_lambda", [DEPTH, 2, D])
    w_proj_rnn = din("w_proj_rnn", [DEPTH, D, D])
    w_proj_fourier = din("w_proj_fourier", [DEPTH, 512, D])
    w_out = din("w_out", [DEPTH, D, D])
    norm_ffn_g = din("norm_ffn_g", [DEPTH, D])
    router_w = din("router_w", [D, NE])
    router_b = din("router_b", [1, NE])
    moe_w1 = din("moe_w1", [DEPTH, NE, D, D])
    moe_w3 = din("moe_w3", [DEPTH, NE, D, D])
    moe_w2 = din("moe_w2", [DEPTH, NE, D, D])
    final_norm_g = din("final_norm_g", [1, D])
    ident_in = din("ident", [128, 128])
    csc_in = din("csc", [128, 256])
    rc_in = din("rconst", [128, 448])
    dftl_in = din("dftl", [2, L, L], BF16)
    dftc_in = din("dftc", [2, LC, LC], BF16)
    out_d = nc.dram_tensor("out", [L, D], F32, kind="ExternalOutput").ap()

    def mk_stream(name, Ln, T, on_grid, src):
        s = Stream()
        s.name, s.L, s.T, s.on_grid, s.src = name, Ln, T, on_grid, src
        s.nt = Ln // T
        s.nsub = T // 128
        s.xres = kb.dram("xres_" + name, [Ln, D], F32)
        s.us = kb.dram("us_" + name, [128, 8, Ln], F32)
        s.hf = kb.dram("hf_" + name, [128, 8, Ln], F32)
        s.hxT = kb.dram("hxT_" + name, [128, 8, Ln], BF16)
        s.AB = kb.dram("AB_" + name, [Ln, D], BF16)
        s.yfT = kb.dram("yfT_" + name, [128, 4, Ln], BF16)
        s.b_xres = [Buf() for _ in range(s.nt)]
        s.b_us = [Buf() for _ in range(s.nt)]
        s.b_hf = [Buf() for _ in range(s.nt)]
        s.b_hxT = [Buf() for _ in range(s.nt)]
        s.b_AB = [Buf() for _ in range(s.nt)]
        s.b_yfT = [Buf() for _ in range(s.nt)]
        s.first = True
        return s

    SX = mk_stream("x", L, 512, True, x_in)
    SC = mk_stream("c", LC, 256, False, ctx_in)
    SX.sidx, SC.sidx = 0, 1
    SX.dft, SC.dft = dftl_in, dftc_in
    LT = LC + L
    h2s_d = kb.dram("h2s", [NSLOT, D], BF16)
    ys_d = kb.dram("ys", [NSLOT, D], F32)
    wbf_d = [[kb.dram("wbf_%d_%d" % (l_, m_), [NE * 128, 8 * D], BF16, dumpable=False) for m_ in range(3)]
             for l_ in range(DEPTH)]
    cast_q = {l_: [(m_, e_) for e_ in range(NE) for m_ in range(3)] for l_ in range(DEPTH)}
    cast_l = [0]

    def feed(n=2):
        q_ = cast_q[cast_l[0]]
        wsrc_ = (moe_w1, moe_w3, moe_w2)
        for _ in range(n):
            if not q_:
                return
            m_, e_ = q_.pop(0)
            kb.dma(pool, wbf_d[cast_l[0]][m_][e_ * 128:(e_ + 1) * 128, :],
                   wsrc_[m_][cast_l[0], e_].rearrange("(p c) f -> p (c f)", c=8))

    top = kb.es
    ident = kb.sb(top, "ident", [128, 128], F32)
    b_ident = Buf()
    kb.dma(sp, ident[:], ident_in, writes=[b_ident])
    csc = kb.sb(top, "csc", [128, 256], BF16)
    b_csc = Buf()
    kb.dma(pool, csc[:], csc_in, writes=[b_csc])
    ones = kb.sb(top, "ones", [128, 128], F32)
    b_ones = Buf()
    kb.op(dve, lambda: nc.vector.memset(ones[:], 1.0), writes=[b_ones])
    ccf = kb.sb(top, "ccf", [128, 8, 2], F32)
    b_ccf = Buf()
    with nc.allow_non_contiguous_dma(reason="tiny one-time transposed load"):
        for s in range(2):
            kb.dma(sp, ccf[:, :, s], cc_in[s].rearrange("(c p) -> p c", p=128), writes=[b_ccf])
    kb.op(act, lambda: nc.scalar.activation(out=ccf[:], in_=ccf[:], func=AF.Silu), reads=[b_ccf], writes=[b_ccf])
    rw = kb.sb(top, "rw", [128, 8, 128], F32)
    b_rw = Buf()
    kb.op(dve, lambda: nc.vector.memset(rw[:].rearrange("p c n -> p (c n)"), 0.0), writes=[b_rw])
    kb.dma(sp, rw[:, :, :NE], router_w.rearrange("(c p) n -> p c n", p=128), writes=[b_rw])
    rb_bc = kb.sb(top, "rb_bc", [128, NE], F32)
    b_rb = Buf()
    kb.dma(sp, rb_bc[:], router_b.partition_broadcast(128), writes=[b_rb])
    gF_bc = kb.sb(top, "gF_bc", [128, D], F32)
    b_gF = Buf()
    kb.dma(sp, gF_bc[:], final_norm_g.partition_broadcast(128), writes=[b_gF])
    carry = kb.sb(top, "carry", [128, 2, 8], F32)
    b_carry = [[Buf() for _ in range(8)] for _ in range(2)]
    g2 = kb.sb(top, "g2", [128, 2, D], F32)
    b_g2 = [Buf(), Buf()]
    NSMAX = 34
    rc = kb.sb(top, "rc", [128, 448], F32)
    b_rc = Buf()
    kb.dma(sp, rc[:], rc_in, writes=[b_rc])
    identb = kb.sb(top, "identb", [128, 128], BF16)
    b_identb = Buf()
    kb.op(dve, lambda: nc.vector.tensor_copy(out=identb[:], in_=ident[:]), reads=[b_ident], writes=[b_identb])
    zrow = kb.sb(top, "zrow", [128, D], BF16)
    b_zrow = Buf()
    kb.op(dve, lambda: nc.vector.memset(zrow[:], 0.0), writes=[b_zrow])
    pos2i = kb.sb(top, "pos2i", [128, NSMAX, 2], I32)
    gate2 = kb.sb(top, "gate2", [128, NSMAX, 2], F32)
    widx = kb.sb(top, "widx", [128, 32], I32)
    b_route = Buf()

    banks = []
    for i in range(8):
        t = top.enter_context(nc.psum_tensor("bank%d" % i, [128, 512], F32))
        banks.append((t, Buf("bank%d" % i, psum=True)))
    bank_rr = [0]

    bank_mode = ["all"]
    bank_rr2 = [0]

    def next_bank():
        if bank_mode[0] == "hi":
            b = banks[4 + bank_rr2[0] % 4]
            bank_rr2[0] += 1
            return b
        b = banks[bank_rr[0] % 8]
        bank_rr[0] += 1
        return b

    def mm(bank, out_ap, lhsT, rhs, reads, start, stop, tick=None):
        return kb.pe_op(lambda: nc.tensor.matmul(out_ap, lhsT, rhs, start=start, stop=stop),
                        reads=reads, writes=[bank[1]], tick=(stop if tick is None else tick))

    def mod_phase(l, part, mods, b_mods, gnorm_dram):
        with ExitStack() as st:
            crep = kb.sb(st, "crep", [128, 2, 8, 128], F32)
            b_crep = Buf()
            for s in range(2):
                for c in range(8):
                    kb.op(dve, lambda s=s, c=c: nc.vector.tensor_scalar(
                        out=crep[:, s, c, :], in0=ones[:], scalar1=ccf[:, c, s:s + 1], scalar2=None, op0=ALU.mult),
                        reads=[b_ones, b_ccf], writes=[b_crep])
            adab = kb.sb(st, "adab", [128, 3 * D], F32)
            b_adab = Buf()
            kb.dma(sp, adab[:], ada_b[l:l + 1, part * 3 * D:(part + 1) * 3 * D].partition_broadcast(128),
                   writes=[b_adab])
            gn = kb.sb(st, "gn", [128, D], F32)
            b_gn = Buf()
            kb.dma(sp, gn[:], gnorm_dram[l:l + 1, :].partition_broadcast(128), writes=[b_gn])
            wblk = [kb.sb(st, "wblk", [128, 8, 512], F32) for _ in range(2)]
            b_wblk = [Buf(), Buf()]
            aw = ada_w[l].rearrange("(c p) n -> p c n", p=128)
            for blk in range(6):
                col0 = part * 3 * D + blk * 512
                wb, bwb = wblk[blk % 2], b_wblk[blk % 2]
                kb.dma(sp, wb[:], aw[:, :, col0:col0 + 512], writes=[bwb])
                j, off = blk // 2, (blk % 2) * 512
                for s in range(2):
                    bank = next_bank()
                    for k in range(8):
                        mm(bank, bank[0][:, :], crep[:, s, k, :], wb[:, k, :], [b_crep, bwb], k == 0, k == 7)
                    kb.op(dve, lambda s=s, j=j, off=off, bank=bank, blk=blk: nc.vector.tensor_tensor(
                        out=mods[:, s, j, off:off + 512], in0=bank[0][:, :], in1=adab[:, blk * 512:(blk + 1) * 512],
                        op=ALU.add), reads=[bank[1], b_adab], writes=[b_mods[s][j]])
            for s in range(2):
                kb.op(dve, lambda s=s: nc.vector.scalar_tensor_tensor(
                    out=mods[:, s, 1, :], in0=mods[:, s, 1, :], scalar=1.0, in1=gn[:], op0=ALU.add, op1=ALU.mult),
                    reads=[b_gn, b_mods[s][1]], writes=[b_mods[s][1]])
            kb.barrier()

    def load_x_sub(S, ti, s, xt, b_xt):
        r0 = ti * S.T + s * 128
        src = S.src if S.first else S.xres
        kb.dma(sp, xt[:], src[r0:r0 + 128, :], reads=([] if S.first else [S.b_xres[ti]]), writes=[b_xt])

    def norm_sub(xt, b_xt, hx, b_hx, Gt, b_G, St, b_S, sc, b_sc):
        kb.op(act, lambda: nc.scalar.activation(out=hx[:], in_=xt[:], func=AF.Square, accum_out=sc),
              reads=[b_xt], writes=[b_hx, b_sc])
        kb.op(dve, lambda: nc.vector.tensor_scalar(out=sc, in0=sc, scalar1=1.0 / D, scalar2=EPS, op0=ALU.mult,
                                                   op1=ALU.add), reads=[b_sc], writes=[b_sc])
        kb.op(act, lambda: nc.scalar.activation(out=sc, in_=sc, func=AF.Ln), reads=[b_sc], writes=[b_sc])
        kb.op(act, lambda: nc.scalar.activation(out=sc, in_=sc, func=AF.Exp, scale=-0.5), reads=[b_sc], writes=[b_sc])
        kb.op(dve, lambda: nc.vector.scalar_tensor_tensor(out=hx[:], in0=xt[:], scalar=sc, in1=Gt, op0=ALU.mult,
                                                          op1=ALU.mult), reads=[b_xt, b_sc, b_G], writes=[b_hx])
        kb.op(dve, lambda: nc.vector.tensor_tensor(out=hx[:], in0=hx[:], in1=St, op=ALU.add),
              reads=[b_S, b_hx], writes=[b_hx])

    def transpose_sub(hx, b_hx, s, outs):
        for half in range(2):
            bank = next_bank()
            for cc in range(4):
                c = half * 4 + cc
                kb.pe_op(lambda c=c, cc=cc, bank=bank: nc.tensor.transpose(
                    out=bank[0][:, cc * 128:(cc + 1) * 128], in_=hx[:, c * 128:(c + 1) * 128], identity=ident[:]),
                    reads=[b_hx, b_ident], writes=[bank[1]], tick=(cc == 3))
            pv = bank[0][:, :].rearrange("p (c t) -> p c t", t=128)
            srcv, srcb = pv, [bank[1]]
            for (q, tl, bufs) in outs:
                dst = tl[:, half * 4:(half + 1) * 4, s * 128:(s + 1) * 128]
                wb = bufs[half * 4:(half + 1) * 4]
                if q is act:
                    kb.op(act, lambda dst=dst, srcv=srcv: nc.scalar.copy(out=dst, in_=srcv), reads=srcb, writes=wb)
                else:
                    kb.op(dve, lambda dst=dst, srcv=srcv: nc.vector.tensor_copy(out=dst, in_=srcv), reads=srcb,
                          writes=wb)
                srcv, srcb = dst, wb

    def norm_transpose_tile(S, ti, wkn, G, bG, Sh, bSh, outs, tok_out=None):
        xts, b_xts, hxs, b_hxs, ss, b_ss, cnt = wkn
        for s0 in range(0, S.nsub, 2):
            pr = [(s0 + j, j) for j in range(2) if s0 + j < S.nsub]
            for (s, i) in pr:
                load_x_sub(S, ti, s, xts[i], b_xts[i])
            sc = lambda i: ss[:, i:i + 1]
            for (s, i) in pr:
                kb.op(act, lambda i=i: nc.scalar.activation(out=hxs[i][:], in_=xts[i][:], func=AF.Square,
                                                            accum_out=sc(i)), reads=[b_xts[i]],
                      writes=[b_hxs[i], b_ss[i]])
            for (s, i) in pr:
                kb.op(dve, lambda i=i: nc.vector.tensor_scalar(out=sc(i), in0=sc(i), scalar1=1.0 / D, scalar2=EPS,
                                                               op0=ALU.mult, op1=ALU.add), reads=[b_ss[i]],
                      writes=[b_ss[i]])
            for (s, i) in pr:
                kb.op(act, lambda i=i: nc.scalar.activation(out=sc(i), in_=sc(i), func=AF.Ln), reads=[b_ss[i]],
                      writes=[b_ss[i]])
            for (s, i) in pr:
                kb.op(act, lambda i=i: nc.scalar.activation(out=sc(i), in_=sc(i), func=AF.Exp, scale=-0.5),
                      reads=[b_ss[i]], writes=[b_ss[i]])
            for (s, i) in pr:
                kb.op(dve, lambda i=i: nc.vector.scalar_tensor_tensor(out=hxs[i][:], in0=xts[i][:], scalar=sc(i),
                                                                      in1=G, op0=ALU.mult, op1=ALU.mult),
                      reads=[b_xts[i], b_ss[i], bG], writes=[b_hxs[i]])
            for (s, i) in pr:
                kb.op(dve, lambda i=i: nc.vector.tensor_tensor(out=hxs[i][:], in0=hxs[i][:], in1=Sh, op=ALU.add),
                      reads=[bSh, b_hxs[i]], writes=[b_hxs[i]])
            if tok_out is not None:
                tk, b_tk, sbase = tok_out
                for (s, i) in pr:
                    kb.op(act, lambda s=s, i=i: nc.scalar.copy(out=tk[:, sbase + s, :], in_=hxs[i][:]),
                          reads=[b_hxs[i]], writes=[b_tk[sbase + s]])
            for (s, i) in pr:
                transpose_sub(hxs[i], b_hxs[i], s, outs)

    def norm_work(st):
        xts = [kb.sb(st, "xts", [128, D], F32) for _ in range(2)]
        hxs = [kb.sb(st, "hxs", [128, D], F32) for _ in range(2)]
        ss = kb.sb(st, "ss", [128, 2], F32)
        return (xts, [Buf(), Buf()], hxs, [Buf(), Buf()], ss, [Buf(), Buf()], [0])

    def scan_dir(S, d, T, lw, u, b_u, ubf, b_ubf, h, b_h, gk, reverse):
        lruw, b_lruw, lb, b_lb, nls, b_nls = lw
        r_all, i_all, a_all, b_r, b_i, b_a = gk
        A = lambda fn, rd, wr: kb.op(act, fn, reads=rd, writes=wr)
        V = lambda fn, rd, wr: kb.op(dve, fn, reads=rd, writes=wr)
        G = 4
        for c0 in range(0, 8, G):
            cs = list(range(c0, c0 + G))
            bks = {}
            for c in cs:
                bk_r = next_bank()
                mm(bk_r, bk_r[0][:, :T], lruw[:, d, 0, c, :], ubf[:, c, :T], [b_lruw, b_ubf[c]], True, True)
                bk_i = next_bank()
                mm(bk_i, bk_i[0][:, :T], lruw[:, d, 1, c, :], ubf[:, c, :T], [b_lruw, b_ubf[c]], True, True)
                bks[c] = (bk_r, bk_i)
            R = lambda c: r_all[:, c, :T]
            I = lambda c: i_all[:, c, :T]
            AA = lambda c: a_all[:, c, :T]
            for c in cs:
                A(lambda c=c: nc.scalar.activation(out=R(c), in_=bks[c][0][0][:, :T], func=AF.Exp, scale=-1.0,
                                                   bias=lb[:, d, 0, c:c + 1]), [bks[c][0][1], b_lb], [b_r[c]])
                A(lambda c=c: nc.scalar.activation(out=I(c), in_=bks[c][1][0][:, :T], func=AF.Exp, scale=-1.0,
                                                   bias=lb[:, d, 1, c:c + 1]), [bks[c][1][1], b_lb], [b_i[c]])
            for c in cs:
                A(lambda c=c: nc.scalar.activation(out=R(c), in_=R(c), func=AF.Ln, bias=1.0), [b_r[c]], [b_r[c]])
                A(lambda c=c: nc.scalar.activation(out=I(c), in_=I(c), func=AF.Ln, bias=1.0), [b_i[c]], [b_i[c]])
            for c in cs:
                A(lambda c=c: nc.scalar.activation(out=R(c), in_=R(c), func=AF.Exp, scale=-1.0), [b_r[c]], [b_r[c]])
                A(lambda c=c: nc.scalar.activation(out=I(c), in_=I(c), func=AF.Exp, scale=-1.0), [b_i[c]], [b_i[c]])
            for c in cs:
                A(lambda c=c: nc.scalar.activation(out=AA(c), in_=R(c), func=AF.Exp, scale=nls[:, d, 0, c:c + 1]),
                  [b_r[c], b_nls], [b_a[c]])
            for c in cs:
                A(lambda c=c: nc.scalar.activation(out=R(c), in_=R(c), func=AF.Exp, scale=nls[:, d, 1, c:c + 1]),
                  [b_r[c], b_nls], [b_r[c]])
                V(lambda c=c: nc.vector.tensor_tensor(out=I(c), in0=I(c), in1=u[:, c, :T], op=ALU.mult),
                  [b_i[c], b_u[c]], [b_i[c]])
            for c in cs:
                A(lambda c=c: nc.scalar.activation(out=R(c), in_=R(c), func=AF.Ln, scale=-1.0, bias=1.0),
                  [b_r[c]], [b_r[c]])
            for c in cs:
                A(lambda c=c: nc.scalar.activation(out=R(c), in_=R(c), func=AF.Exp, scale=0.5), [b_r[c]], [b_r[c]])
            for c in cs:
                V(lambda c=c: nc.vector.tensor_tensor(out=I(c), in0=I(c), in1=R(c), op=ALU.mult),
                  [b_i[c], b_r[c]], [b_i[c]])
            for c in cs:
                o_ap, a_ap, b_ap = h[:, c, :T], AA(c), I(c)
                if reverse:
                    o_ap, a_ap, b_ap = rev_last(o_ap), rev_last(a_ap), rev_last(b_ap)
                V(lambda c=c, o_ap=o_ap, a_ap=a_ap, b_ap=b_ap: nc.vector.tensor_tensor_scan(
                    out=o_ap, data0=a_ap, data1=b_ap, initial=carry[:, d, c:c + 1], op0=ALU.mult, op1=ALU.add),
                    [b_a[c], b_i[c], b_carry[d][c]], [b_h[c]])
            lastcol = 0 if reverse else T - 1
            for c in cs:
                V(lambda c=c: nc.vector.tensor_copy(out=carry[:, d, c:c + 1], in_=h[:, c, lastcol:lastcol + 1]),
                  [b_h[c]], [b_carry[d][c]])

    def load_lru(st, l):
        lruw = kb.sb(st, "lruw", [128, 2, 2, 8, 128], BF16)
        b_lruw = Buf()
        for d in range(2):
            kb.dma(pool, lruw[:, d, 0], lru_w_a[l, d].rearrange("h i j -> i h j"), writes=[b_lruw])
            kb.dma(pool, lruw[:, d, 1], lru_w_x[l, d].rearrange("h i j -> i h j"), writes=[b_lruw])
        lb = kb.sb(st, "lb", [128, 2, 2, 8], F32)
        b_lb = Buf()
        nls = kb.sb(st, "nls", [128, 2, 2, 8], F32)
        b_nls = Buf()
        with nc.allow_non_contiguous_dma(reason="tiny per-partition parameter loads"):
            for d in range(2):
                kb.dma(sp, lb[:, d, 0, :], lru_b_a[l, d].rearrange("h j -> j h"), writes=[b_lb])
                kb.dma(sp, lb[:, d, 1, :], lru_b_x[l, d].rearrange("h j -> j h"), writes=[b_lb])
                kb.dma(sp, nls[:, d, 0, :], lru_lambda[l, d].rearrange("(h j) -> j h", j=128), writes=[b_nls])
        kb.op(dve, lambda: nc.vector.tensor_scalar(out=lb[:].rearrange("p a b c -> p (a b c)"),
                                                   in0=lb[:].rearrange("p a b c -> p (a b c)"), scalar1=-1.0,
                                                   scalar2=None, op0=ALU.mult), reads=[b_lb], writes=[b_lb])
        for d in range(2):
            kb.op(act, lambda d=d: nc.scalar.activation(out=nls[:, d, 0, :], in_=nls[:, d, 0, :], func=AF.Exp,
                                                        scale=-1.0), reads=[b_nls], writes=[b_nls])
        for d in range(2):
            kb.op(act, lambda d=d: nc.scalar.activation(out=nls[:, d, 0, :], in_=nls[:, d, 0, :], func=AF.Ln,
                                                        bias=1.0), reads=[b_nls], writes=[b_nls])
        for d in range(2):
            kb.op(dve, lambda d=d: nc.vector.tensor_scalar(out=nls[:, d, 1, :], in0=nls[:, d, 0, :], scalar1=-16.0,
                                                           scalar2=None, op0=ALU.mult), reads=[b_nls], writes=[b_nls])
            kb.op(dve, lambda d=d: nc.vector.tensor_scalar(out=nls[:, d, 0, :], in0=nls[:, d, 0, :], scalar1=-8.0,
                                                           scalar2=None, op0=ALU.mult), reads=[b_nls], writes=[b_nls])
        return (lruw, b_lruw, lb, b_lb, nls, b_nls)

    def gate_work(st, Tw=512):
        r_all = kb.sb(st, "r_all", [128, 8, Tw], F32)
        i_all = kb.sb(st, "i_all", [128, 8, Tw], F32)
        a_all = kb.sb(st, "a_all", [128, 8, Tw], F32)
        return (r_all, i_all, a_all, [Buf() for _ in range(8)], [Buf() for _ in range(8)],
                [Buf() for _ in range(8)])

    def sweep1(S, l, mods, b_mods, do_four):
        T, nsub = S.T, S.nsub
        with ExitStack() as st:
            win1 = kb.sb(st, "win1", [128, 8, 1536], BF16)
            b_win1 = Buf()
            wv = w_in[l].rearrange("(c p) n -> p c n", p=128)
            kb.dma(pool, win1[:, :, 0:1024], wv[:, :, 0:1024], writes=[b_win1])
            kb.dma(pool, win1[:, :, 1024:1536], wv[:, :, 2048:2560], writes=[b_win1])
            lw = load_lru(st, l)
            cw = kb.sb(st, "cw", [128, 8, 4], F32)
            cb = kb.sb(st, "cb", [128, 8], F32)
            b_cw = Buf()
            with nc.allow_non_contiguous_dma(reason="tiny per-partition parameter loads"):
                for k in range(4):
                    kb.dma(sp, cw[:, :, k], conv_w[l, k].rearrange("(c p) -> p c", p=128), writes=[b_cw])
                kb.dma(sp, cb[:], conv_b[l].rearrange("(c p) -> p c", p=128), writes=[b_cw])
            wkn = norm_work(st)
            hxT = kb.sb(st, "hxT", [128, 8, 512], BF16)
            b_hxT = [Buf() for _ in range(8)]
            u = kb.sb(st, "u", [128, 8, 512], F32)
            b_u = [Buf() for _ in range(8)]
            ubf = kb.sb(st, "ubf", [128, 8, 512], BF16)
            b_ubf = [Buf() for _ in range(8)]
            u4 = kb.sb(st, "u4", [128, 4, 512], BF16)
            b_u4 = [Buf() for _ in range(4)]
            abt = kb.sb(st, "abt", [128, 4, D], BF16)
            b_abt = [Buf() for _ in range(4)]
            gk = gate_work(st)
            hf, b_hf = gk[0], gk[3]
            G, bG, Sh, bSh = mods[:, S.sidx, 1, :], b_mods[S.sidx][1], mods[:, S.sidx, 0, :], b_mods[S.sidx][0]
            R = T // 64 if S.on_grid else 1
            W = 64 if S.on_grid else T
            for ti in range(S.nt):
                feed(3)
                norm_transpose_tile(S, ti, wkn, G, bG, Sh, bSh, [(act, hxT, b_hxT)])
                kb.dma(pool, S.hxT[:, :, ti * T:(ti + 1) * T], hxT[:, :, :T], reads=b_hxT, writes=[S.b_hxT[ti]])
                for c0 in range(0, 8, 4):
                    cs = list(range(c0, c0 + 4))
                    bk = {}
                    for c in cs:
                        bank = next_bank()
                        for k in range(8):
                            mm(bank, bank[0][:, :T], win1[:, k, c * 128:(c + 1) * 128], hxT[:, k, :T],
                               [b_win1, b_hxT[k]], k == 0, k == 7)
                        bk[c] = bank
                    PV = lambda c: bk[c][0][:, :T].rearrange("p (r w) -> p r w", w=W)
                    UV = lambda c: u[:, c, :T].rearrange("p (r w) -> p r w", w=W)
                    for c in cs:
                        kb.op(act, lambda c=c: nc.scalar.activation(
                            out=u[:, c, :T], in_=bk[c][0][:, :T], func=AF.Identity, scale=cw[:, c, 2:3],
                            bias=cb[:, c:c + 1]), reads=[bk[c][1], b_cw], writes=[b_u[c]])
                    for (k, so, do, n) in ((1, 0, 1, W - 1), (0, 0, 2, W - 2), (3, 1, 0, W - 1)):
                        for c in cs:
                            kb.op(dve, lambda c=c, k=k, so=so, do=do, n=n: nc.vector.scalar_tensor_tensor(
                                out=UV(c)[:, :, do:do + n], in0=PV(c)[:, :, so:so + n], scalar=cw[:, c, k:k + 1],
                                in1=UV(c)[:, :, do:do + n], op0=ALU.mult, op1=ALU.add),
                                reads=[bk[c][1], b_cw, b_u[c]], writes=[b_u[c]])
                    for c in cs:
                        kb.op(act, lambda c=c: nc.scalar.copy(out=ubf[:, c, :T], in_=u[:, c, :T]),
                              reads=[b_u[c]], writes=[b_ubf[c]])
                kb.dma(pool, S.us[:, :, ti * T:(ti + 1) * T], u[:, :, :T], reads=b_u, writes=[S.b_us[ti]])
                if do_four:
                    for g in range(4):
                        bank = next_bank()
                        for k in range(8):
                            mm(bank, bank[0][:, :T], win1[:, k, 1024 + g * 128:1024 + (g + 1) * 128], hxT[:, k, :T],
                               [b_win1, b_hxT[k]], k == 0, k == 7)
                        kb.op(act, lambda g=g, bank=bank: nc.scalar.copy(out=u4[:, g, :T], in_=bank[0][:, :T]),
                              reads=[bank[1]], writes=[b_u4[g]])
                    for s in range(nsub):
                        for g in range(4):
                            bank = next_bank()
                            mm(bank, bank[0][:, :256], u4[:, g, s * 128:(s + 1) * 128], csc[:, :],
                               [b_u4[g], b_csc], True, True)
                            kb.op(dve, lambda s=s, g=g, bank=bank: nc.vector.tensor_copy(
                                out=abt[:, s, g * 256:(g + 1) * 256], in_=bank[0][:, :256]),
                                reads=[bank[1]], writes=[b_abt[s]])
                    kb.dma(pool, S.AB[ti * T:(ti + 1) * T, :].rearrange("(s p) n -> p s n", p=128), abt[:, :nsub, :],
                           reads=b_abt[:nsub], writes=[S.b_AB[ti]])
                scan_dir(S, 0, T, lw, u, b_u, ubf, b_ubf, hf, b_hf, gk, False)
                kb.dma(pool, S.hf[:, :, ti * T:(ti + 1) * T], hf[:, :, :T], reads=b_hf, writes=[S.b_hf[ti]])
            kb.barrier()

    def sweep2a(S, l, need_h):
        T = S.T
        with ExitStack() as st:
            lw = load_lru(st, l)
            u = kb.sb(st, "u", [128, 8, 512], F32)
            b_u = [Buf() for _ in range(8)]
            b_uall = Buf()
            ubf = kb.sb(st, "ubf", [128, 8, 512], BF16)
            b_ubf = [Buf() for _ in range(8)]
            hf = kb.sb(st, "hft", [128, 8, 512], F32)
            b_hfa = Buf()
            gk = gate_work(st)
            hb, b_hb = gk[0], gk[3]
            for ti in reversed(range(S.nt)):
                kb.dma(sp, u[:, :, :T], S.us[:, :, ti * T:(ti + 1) * T], reads=[S.b_us[ti]], writes=b_u)
                for c in range(8):
                    kb.op(act, lambda c=c: nc.scalar.copy(out=ubf[:, c, :T], in_=u[:, c, :T]),
                          reads=[b_u[c]], writes=[b_ubf[c]])
                scan_dir(S, 1, T, lw, u, b_u, ubf, b_ubf, hb, b_hb, gk, True)
                if need_h:
                    kb.dma(sp, hf[:, :, :T], S.hf[:, :, ti * T:(ti + 1) * T], reads=[S.b_hf[ti]], writes=[b_hfa])
                    for c in range(8):
                        kb.op(dve, lambda c=c: nc.vector.tensor_tensor(out=hb[:, c, :T], in0=hb[:, c, :T],
                                                                       in1=hf[:, c, :T], op=ALU.add),
                              reads=[b_hfa, b_hb[c]], writes=[b_hb[c]])
                    kb.dma(pool, S.hf[:, :, ti * T:(ti + 1) * T], hb[:, :, :T], reads=b_hb, writes=[S.b_hf[ti]])
            kb.barrier()

    def dft_phase(S):
        nch = S.L // 128
        Tb = S.T
        nblk = S.L // Tb
        pc = min(8, nch)
        npiece = nch // pc
        with ExitStack() as st:
            ab = kb.sb(st, "ab_all", [128, nch, D], BF16)
            b_ab = Buf()
            for ti in range(S.nt):
                n0 = ti * S.nsub
                kb.dma(sp, ab[:, n0:n0 + S.nsub, :],
                       S.AB[ti * S.T:(ti + 1) * S.T, :].rearrange("(s p) n -> p s n", p=128),
                       reads=[S.b_AB[ti]], writes=[b_ab])
            tabs = [kb.sb(st, "tab", [128, pc, 2, Tb], BF16) for _ in range(3)]
            b_tabs = [Buf() for _ in range(3)]
            yft = [kb.sb(st, "yft", [128, 4, Tb], BF16) for _ in range(2)]
            b_yft = [Buf(), Buf()]
            tv = [S.dft[k].rearrange("(c p) n -> p c n", p=128) for k in range(2)]
            pi = 0
            for j in range(nblk):
                bks = [next_bank() for _ in range(4)]
                for q in range(npiece):
                    tab, b_tab = tabs[pi % 3], b_tabs[pi % 3]
                    pi += 1
                    for k in range(2):
                        kb.dma(sp, tab[:, :, k, :], tv[k][:, q * pc:(q + 1) * pc, j * Tb:(j + 1) * Tb],
                               writes=[b_tab])
                    for cc in range(pc):
                        c = q * pc + cc
                        for g in range(4):
                            mm(bks[g], bks[g][0][:, :Tb], ab[:, c, g * 256:g * 256 + 128], tab[:, cc, 0, :],
                               [b_ab, b_tab], c == 0, False)
                            mm(bks[g], bks[g][0][:, :Tb], ab[:, c, g * 256 + 128:g * 256 + 256], tab[:, cc, 1, :],
                               [b_ab, b_tab], False, c == nch - 1, tick=(c == nch - 1 or (cc == pc - 1 and g == 3)))
                y, b_y = yft[j % 2], b_yft[j % 2]
                for g in range(4):
                    kb.op(act, lambda g=g, y=y, bks=bks: nc.scalar.copy(out=y[:, g, :], in_=bks[g][0][:, :Tb]),
                          reads=[bks[g][1]], writes=[b_y])
                kb.dma(pool, S.yfT[:, :, j * Tb:(j + 1) * Tb], y[:, :, :], reads=[b_y], writes=[S.b_yfT[j]])
            kb.barrier()

    def scan_group(d, T, lw, cs, u_t, b_ut, ubf_t, b_ubft, gw, reverse):
        lruw, b_lruw, lb, b_lb, nls, b_nls = lw
        r_t, i_t, a_t, b_rt, b_it, b_at = gw
        A = lambda fn, rd, wr: kb.op(act, fn, reads=rd, writes=wr)
        V = lambda fn, rd, wr: kb.op(dve, fn, reads=rd, writes=wr)
        n = len(cs)
        bks = []
        for j, c in enumerate(cs):
            bk_r = next_bank()
            mm(bk_r, bk_r[0][:, :T], lruw[:, d, 0, c, :], ubf_t[:, j, :T], [b_lruw, b_ubft[j]], True, True)
            bk_i = next_bank()
            mm(bk_i, bk_i[0][:, :T], lruw[:, d, 1, c, :], ubf_t[:, j, :T], [b_lruw, b_ubft[j]], True, True)
            bks.append((bk_r, bk_i))
        R = lambda j: r_t[:, j, :T]
        I = lambda j: i_t[:, j, :T]
        AA = lambda j: a_t[:, j, :T]
        for j, c in enumerate(cs):
            A(lambda j=j, c=c: nc.scalar.activation(out=R(j), in_=bks[j][0][0][:, :T], func=AF.Exp, scale=-1.0,
                                                    bias=lb[:, d, 0, c:c + 1]), [bks[j][0][1], b_lb], [b_rt[j]])
            A(lambda j=j, c=c: nc.scalar.activation(out=I(j), in_=bks[j][1][0][:, :T], func=AF.Exp, scale=-1.0,
                                                    bias=lb[:, d, 1, c:c + 1]), [bks[j][1][1], b_lb], [b_it[j]])
        for j in range(n):
            A(lambda j=j: nc.scalar.activation(out=R(j), in_=R(j), func=AF.Ln, bias=1.0), [b_rt[j]], [b_rt[j]])
            A(lambda j=j: nc.scalar.activation(out=I(j), in_=I(j), func=AF.Ln, bias=1.0), [b_it[j]], [b_it[j]])
        for j in range(n):
            A(lambda j=j: nc.scalar.activation(out=R(j), in_=R(j), func=AF.Exp, scale=-1.0), [b_rt[j]], [b_rt[j]])
            A(lambda j=j: nc.scalar.activation(out=I(j), in_=I(j), func=AF.Exp, scale=-1.0), [b_it[j]], [b_it[j]])
        for j, c in enumerate(cs):
            A(lambda j=j, c=c: nc.scalar.activation(out=AA(j), in_=R(j), func=AF.Exp, scale=nls[:, d, 0, c:c + 1]),
              [b_rt[j], b_nls], [b_at[j]])
        for j, c in enumerate(cs):
            A(lambda j=j, c=c: nc.scalar.activation(out=R(j), in_=R(j), func=AF.Exp, scale=nls[:, d, 1, c:c + 1]),
              [b_rt[j], b_nls], [b_rt[j]])
            V(lambda j=j: nc.vector.tensor_tensor(out=I(j), in0=I(j), in1=u_t[:, j, :T], op=ALU.mult),
              [b_it[j], b_ut[j]], [b_it[j]])
        for j in range(n):
            A(lambda j=j: nc.scalar.activation(out=R(j), in_=R(j), func=AF.Ln, scale=-1.0, bias=1.0),
              [b_rt[j]], [b_rt[j]])
        for j in range(n):
            A(lambda j=j: nc.scalar.activation(out=R(j), in_=R(j), func=AF.Exp, scale=0.5), [b_rt[j]], [b_rt[j]])
        for j in range(n):
            V(lambda j=j: nc.vector.tensor_tensor(out=I(j), in0=I(j), in1=R(j), op=ALU.mult),
              [b_it[j], b_rt[j]], [b_it[j]])
        for j, c in enumerate(cs):
            o_ap, a_ap, b_ap = R(j), AA(j), I(j)
            if reverse:
                o_ap, a_ap, b_ap = rev_last(o_ap), rev_last(a_ap), rev_last(b_ap)
            V(lambda c=c, o_ap=o_ap, a_ap=a_ap, b_ap=b_ap: nc.vector.tensor_tensor_scan(
                out=o_ap, data0=a_ap, data1=b_ap, initial=carry[:, d, c:c + 1], op0=ALU.mult, op1=ALU.add),
                [b_at[j], b_it[j], b_carry[d][c]], [b_rt[j]])
        lastcol = 0 if reverse else T - 1
        for j, c in enumerate(cs):
            V(lambda j=j, c=c: nc.vector.tensor_copy(out=carry[:, d, c:c + 1], in_=r_t[:, j, lastcol:lastcol + 1]),
              [b_rt[j]], [b_carry[d][c]])

    def sweep2a_dft(S, l, need_h, do_dft):
        Th = 256
        nh = S.L // Th
        GC = 2
        with ExitStack() as st:
            lw = load_lru(st, l)
            NB = 3
            u_g = [kb.sb(st, "u_g", [128, GC, Th], F32) for _ in range(NB)]
            b_ug = [[Buf() for _ in range(GC)] for _ in range(NB)]
            ubf_g = [kb.sb(st, "ubf_g", [128, GC, Th], BF16) for _ in range(NB)]
            b_ubfg = [[Buf() for _ in range(GC)] for _ in range(NB)]
            hf_g = [kb.sb(st, "hf_g", [128, GC, Th], F32) for _ in range(NB)]
            b_hfg = [Buf() for _ in range(NB)]
            gws = []
            for _ in range(NB):
                gws.append((kb.sb(st, "r_g", [128, GC, Th], F32), kb.sb(st, "i_g", [128, GC, Th], F32),
                            kb.sb(st, "a_g", [128, GC, Th], F32), [Buf() for _ in range(GC)],
                            [Buf() for _ in range(GC)], [Buf() for _ in range(GC)]))
            groups = [(hi, c0) for hi in reversed(range(nh)) for c0 in range(0, 8, GC)]

            def g_pro(gi):
                hi, c0 = groups[gi]
                k = gi % NB
                ti = (hi * Th) // S.T
                cols = slice(hi * Th, (hi + 1) * Th)
                kb.dma(pool, u_g[k][:, :, :], S.us[:, c0:c0 + GC, cols], reads=[S.b_us[ti]], writes=b_ug[k])
                if need_h:
                    kb.dma(pool, hf_g[k][:, :, :], S.hf[:, c0:c0 + GC, cols], reads=[S.b_hf[ti]], writes=[b_hfg[k]])
                kb.dma(pool, ubf_g[k][:, :, :], S.us[:, c0:c0 + GC, cols], reads=[S.b_us[ti]], writes=b_ubfg[k])

            def g_main(gi):
                hi, c0 = groups[gi]
                k = gi % NB
                ti = (hi * Th) // S.T
                cols = slice(hi * Th, (hi + 1) * Th)
                gw = gws[k]
                scan_group(1, Th, lw, list(range(c0, c0 + GC)), u_g[k], b_ug[k], ubf_g[k], b_ubfg[k], gw, True)
                if need_h:
                    for j in range(GC):
                        kb.op(dve, lambda j=j, gw=gw, k=k: nc.vector.tensor_tensor(
                            out=gw[0][:, j, :], in0=gw[0][:, j, :], in1=hf_g[k][:, j, :], op=ALU.add),
                            reads=[b_hfg[k], gw[3][j]], writes=[gw[3][j]])
                    kb.dma(pool, S.hf[:, c0:c0 + GC, cols], gw[0][:, :, :], reads=gw[3], writes=[S.b_hf[ti]])

            ng = len(groups)
            pieces = []
            if do_dft:
                nch = S.L // 128
                Tb = S.T
                nblk = S.L // Tb
                pc = min(8, nch)
                npiece = nch // pc
                ab = kb.sb(st, "ab_all", [128, nch, D], BF16)
                b_ab = Buf()
                for ti in range(S.nt):
                    n0 = ti * S.nsub
                    kb.dma(sp, ab[:, n0:n0 + S.nsub, :],
                           S.AB[ti * S.T:(ti + 1) * S.T, :].rearrange("(s p) n -> p s n", p=128),
                           reads=[S.b_AB[ti]], writes=[b_ab])
                NTB = 3
                tabs = [kb.sb(st, "tab", [128, pc, 2, Tb], BF16) for _ in range(NTB)]
                b_tabs = [Buf() for _ in range(NTB)]
                yft = [kb.sb(st, "yft", [128, 4, Tb], BF16) for _ in range(2)]
                b_yft = [Buf(), Buf()]
                tv = [S.dft[k].rearrange("(c p) n -> p c n", p=128) for k in range(2)]
                bks = banks[0:4]

                def piece(j, q, pidx):
                    tab, b_tab = tabs[pidx % NTB], b_tabs[pidx % NTB]
                    for k in range(2):
                        kb.dma(sp, tab[:, :, k, :], tv[k][:, q * pc:(q + 1) * pc, j * Tb:(j + 1) * Tb],
                               writes=[b_tab])
                    for cc in range(pc):
                        c = q * pc + cc
                        for g in range(4):
                            mm(bks[g], bks[g][0][:, :Tb], ab[:, c, g * 256:g * 256 + 128], tab[:, cc, 0, :],
                               [b_ab, b_tab], c == 0, False)
                            mm(bks[g], bks[g][0][:, :Tb], ab[:, c, g * 256 + 128:g * 256 + 256],
                               tab[:, cc, 1, :], [b_ab, b_tab], False, c == nch - 1,
                               tick=(c == nch - 1 or (cc == pc - 1 and g == 3)))
                    if q == npiece - 1:
                        y, b_y = yft[j % 2], b_yft[j % 2]
                        for g in range(4):
                            kb.op(act, lambda g=g, y=y: nc.scalar.copy(out=y[:, g, :], in_=bks[g][0][:, :Tb]),
                                  reads=[bks[g][1]], writes=[b_y])
                        kb.dma(pool, S.yfT[:, :, j * Tb:(j + 1) * Tb], y[:, :, :], reads=[b_y],
                               writes=[S.b_yfT[j]])

                pidx = 0
                for j in range(nblk):
                    for q in range(npiece):
                        pieces.append(lambda j=j, q=q, pidx=pidx: piece(j, q, pidx))
                        pidx += 1
            bank_mode[0] = "hi"
            nslot = max(len(pieces), 1)
            per = -(-ng // nslot)
            pro_done = 0
            for gi in range(min(NB, ng)):
                g_pro(gi)
                pro_done += 1
            main_done = 0
            for sidx in range(nslot):
                tgt = min(ng, (sidx + 1) * per)
                nxt = min(ng, tgt + per)
                while main_done < tgt:
                    g_main(main_done)
                    main_done += 1
                    if pro_done < ng:
                        g_pro(pro_done)
                        pro_done += 1
                if sidx < len(pieces):
                    pieces[sidx]()
            while main_done < ng:
                g_main(main_done)
                main_done += 1
                if pro_done < ng:
                    g_pro(pro_done)
                    pro_done += 1
            bank_mode[0] = "all"
            kb.barrier()

    def sweep2b(S, l, mods, b_mods):
        T, nsub = S.T, S.nsub
        with ExitStack() as st:
            win2 = kb.sb(st, "win2", [128, 8, 3072], BF16)
            b_win2 = Buf()
            wv = w_in[l].rearrange("(c p) n -> p c n", p=128)
            for (d0, s0) in ((0, 1024), (1024, 2560), (2048, 3584)):
                kb.dma(pool, win2[:, :, d0:d0 + 1024], wv[:, :, s0:s0 + 1024], writes=[b_win2])
            wpr = kb.sb(st, "wpr", [128, 8, D], BF16)
            wpf = kb.sb(st, "wpf", [128, 4, D], BF16)
            wo = kb.sb(st, "wo", [128, 8, D], BF16)
            b_wp = Buf()
            kb.dma(pool, wpr[:], w_proj_rnn[l].rearrange("(c p) n -> p c n", p=128), writes=[b_wp])
            kb.dma(pool, wpf[:], w_proj_fourier[l].rearrange("(c p) n -> p c n", p=128), writes=[b_wp])
            kb.dma(pool, wo[:], w_out[l].rearrange("(c p) n -> p c n", p=128), writes=[b_wp])
            hxT = kb.sb(st, "hxT", [128, 8, 512], BF16)
            b_hxT = Buf()
            hch = [kb.sb(st, "hch", [128, 512], F32) for _ in range(2)]
            b_hch = [Buf(), Buf()]
            yf = kb.sb(st, "yf", [128, 4, 512], BF16)
            b_yf = Buf()
            xs = [kb.sb(st, "xs", [128, D], F32) for _ in range(2)]
            b_xs = [Buf(), Buf()]
            gg = kb.sb(st, "gg", [128, 8, 512], BF16)
            b_gg = [Buf() for _ in range(8)]
            hg = kb.sb(st, "hg", [128, 8, 512], BF16)
            b_hg = [Buf() for _ in range(8)]
            sgr = kb.sb(st, "sgr", [128, 8, 512], BF16)
            b_sgr = [Buf() for _ in range(8)]
            sgf = kb.sb(st, "sgf", [128, 8, 512], BF16)
            b_sgf = [Buf() for _ in range(8)]
            mrg = kb.sb(st, "mrg", [128, 8, 512], BF16)
            b_mrg = [Buf() for _ in range(8)]
            t1 = [kb.sb(st, "t1", [128, 512], F32) for _ in range(2)]
            b_t1 = [Buf(), Buf()]
            t2 = [kb.sb(st, "t2", [128, 512], F32) for _ in range(2)]
            b_t2 = [Buf(), Buf()]
            g1, b_g1 = mods[:, S.sidx, 2, :], b_mods[S.sidx][2]
            for ti in range(S.nt):
                feed(3)
                sl = slice(ti * T, (ti + 1) * T)
                kb.dma(sp, hxT[:, :, :T], S.hxT[:, :, sl], reads=[S.b_hxT[ti]], writes=[b_hxT])
                kb.dma(sp, yf[:, :, :T], S.yfT[:, :, sl], reads=[S.b_yfT[ti]], writes=[b_yf])
                for c in range(8):
                    bank = next_bank()
                    for k in range(8):
                        mm(bank, bank[0][:, :T], win2[:, k, c * 128:(c + 1) * 128], hxT[:, k, :T],
                           [b_win2, b_hxT], k == 0, k == 7)
                    kb.op(act, lambda c=c, bank=bank: nc.scalar.activation(out=gg[:, c, :T], in_=bank[0][:, :T],
                                                                          func=GELU), reads=[bank[1]],
                          writes=[b_gg[c]])
                    hc, bhc = hch[c % 2], b_hch[c % 2]
                    kb.dma(sp, hc[:, :T], S.hf[:, c, sl], reads=[S.b_hf[ti]], writes=[bhc])
                    kb.op(dve, lambda c=c, hc=hc: nc.vector.tensor_tensor(out=hg[:, c, :T], in0=gg[:, c, :T],
                                                                          in1=hc[:, :T], op=ALU.mult),
                          reads=[b_gg[c], bhc], writes=[b_hg[c]])
                for (off, dst, bd) in ((1024, sgr, b_sgr), (2048, sgf, b_sgf)):
                    for c in range(8):
                        bank = next_bank()
                        for k in range(8):
                            mm(bank, bank[0][:, :T], win2[:, k, off + c * 128:off + (c + 1) * 128], hxT[:, k, :T],
                               [b_win2, b_hxT], k == 0, k == 7)
                        kb.op(act, lambda c=c, bank=bank, dst=dst: nc.scalar.activation(
                            out=dst[:, c, :T], in_=bank[0][:, :T], func=AF.Sigmoid), reads=[bank[1]],
                            writes=[bd[c]])
                for j0 in range(0, 8, 2):
                    for j in (j0, j0 + 1):
                        bk_r = next_bank()
                        for k in range(8):
                            mm(bk_r, bk_r[0][:, :T], wpr[:, k, j * 128:(j + 1) * 128], hg[:, k, :T],
                               [b_wp, b_hg[k]], k == 0, k == 7)
                        bk_f = next_bank()
                        for g in range(4):
                            mm(bk_f, bk_f[0][:, :T], wpf[:, g, j * 128:(j + 1) * 128], yf[:, g, :T],
                               [b_wp, b_yf], g == 0, g == 3)
                        a1, ba1, a2, ba2 = t1[j % 2], b_t1[j % 2], t2[j % 2], b_t2[j % 2]
                        kb.op(dve, lambda j=j, bk=bk_r, a1=a1: nc.vector.tensor_tensor(
                            out=a1[:, :T], in0=bk[0][:, :T], in1=sgr[:, j, :T], op=ALU.mult),
                            reads=[bk_r[1], b_sgr[j]], writes=[ba1])
                        kb.op(dve, lambda j=j, bk=bk_f, a2=a2: nc.vector.tensor_tensor(
                            out=a2[:, :T], in0=bk[0][:, :T], in1=sgf[:, j, :T], op=ALU.mult),
                            reads=[bk_f[1], b_sgf[j]], writes=[ba2])
                    for j in (j0, j0 + 1):
                        a1, ba1, a2, ba2 = t1[j % 2], b_t1[j % 2], t2[j % 2], b_t2[j % 2]
                        kb.op(dve, lambda j=j, a1=a1, a2=a2: nc.vector.tensor_tensor(
                            out=mrg[:, j, :T], in0=a1[:, :T], in1=a2[:, :T], op=ALU.add),
                            reads=[ba1, ba2], writes=[b_mrg[j]])
                for s in range(nsub):
                    x1, bx1 = xs[s % 2], b_xs[s % 2]
                    r0 = ti * T + s * 128
                    srcx = S.src if S.first else S.xres
                    kb.dma(sp, x1[:], srcx[r0:r0 + 128, :], reads=([] if S.first else [S.b_xres[ti]]), writes=[bx1])
                    for dh in range(2):
                        bank = next_bank()
                        for k in range(8):
                            mm(bank, bank[0][:, :], mrg[:, k, s * 128:(s + 1) * 128], wo[:, k, dh * 512:(dh + 1) * 512],
                               [b_wp, b_mrg[k]], k == 0, k == 7)
                        a1, ba1 = t1[dh], b_t1[dh]
                        kb.op(dve, lambda dh=dh, bank=bank, a1=a1: nc.vector.tensor_tensor(
                            out=a1[:, :], in0=bank[0][:, :], in1=g1[:, dh * 512:(dh + 1) * 512], op=ALU.mult),
                            reads=[bank[1], b_g1], writes=[ba1])
                    for dh in range(2):
                        a1, ba1 = t1[dh], b_t1[dh]
                        kb.op(dve, lambda dh=dh, a1=a1, x1=x1: nc.vector.tensor_tensor(
                            out=x1[:, dh * 512:(dh + 1) * 512], in0=x1[:, dh * 512:(dh + 1) * 512],
                            in1=a1[:, :], op=ALU.add), reads=[ba1, bx1], writes=[bx1])
                    kb.dma(pool, S.xres[r0:r0 + 128, :], x1[:], reads=[bx1], writes=[S.b_xres[ti]])
            S.first = False
            kb.barrier()

    def moe_pre(l, streams, mods, b_mods):
        tiles = []
        NS = sum(S.L // 128 for S in streams)
        NTILE = (2 * NS * 128 + NE * 511) // 512
        assert NTILE <= 32 and NS <= NSMAX
        with ExitStack() as st:
            wkn = norm_work(st)
            hTf = kb.sb(st, "hTf", [128, 8, 512], F32)
            b_hTf = [Buf() for _ in range(8)]
            h2tok = kb.sb(st, "h2tok", [128, NS, D], BF16)
            b_h2tok = [Buf() for _ in range(NS)]
            lg = kb.sb(st, "lg", [128, NS, NE], F32)
            b_lg = Buf()
            m0, sub0 = 0, 0
            for S in streams:
                G, bG, Sh, bSh = mods[:, S.sidx, 1, :], b_mods[S.sidx][1], mods[:, S.sidx, 0, :], b_mods[S.sidx][0]
                for ti in range(S.nt):
                    T = S.T
                    norm_transpose_tile(S, ti, wkn, G, bG, Sh, bSh, [(dve, hTf, b_hTf)],
                                        tok_out=(h2tok, b_h2tok, sub0))
                    subs = []
                    for s in range(S.nsub):
                        bank = next_bank()
                        for k in range(8):
                            mm(bank, bank[0][:, :NE], hTf[:, k, s * 128:(s + 1) * 128], rw[:, k, :NE],
                               [b_hTf[k], b_rw], k == 0, k == 7)
                        kb.op(dve, lambda bank=bank, si=sub0 + s: nc.vector.tensor_copy(out=lg[:, si, :],
                                                                                      in_=bank[0][:, :NE]),
                              reads=[bank[1]], writes=[b_lg])
                        subs.append(sub0 + s)
                    tiles.append(dict(S=S, ti=ti, m0=m0, T=T, subs=subs))
                    m0 += T
                    sub0 += S.nsub
            mk3 = lambda nm: kb.sb(st, nm, [128, NS, NE], F32)
            aff, sel, sel2, Mk, gts, Cn, Wn, off, posf, hiM, tmp = [mk3(n_) for n_ in (
                "aff", "sel", "sel2", "Mk", "gts", "Cn", "Wn", "off", "posf", "hiM", "tmp")]
            m1 = kb.sb(st, "m1", [128, NS * 4], F32)
            m2 = kb.sb(st, "m2", [128, NS * 4], F32)
            gs = kb.sb(st, "gs", [128, NS * 4], F32)
            gmax = kb.sb(st, "gmax", [128, NS], F32)
            ws = kb.sb(st, "ws", [128, NS], F32)
            phi = kb.sb(st, "phi", [128, NS], F32)
            plo = kb.sb(st, "plo", [128, NS], F32)
            pos2f = kb.sb(st, "pos2f", [128, NS, 2], F32)
            tot = kb.sb(st, "tot", [128, NE], F32)
            ntl = kb.sb(st, "ntl", [128, NE], F32)
            baseT = kb.sb(st, "baseT", [128, NE], F32)
            endT = kb.sb(st, "endT", [128, NE], F32)
            base = kb.sb(st, "base", [128, NE], F32)
            cmp = kb.sb(st, "cmp", [128, NE, 16], F32)
            cmp2 = kb.sb(st, "cmp2", [128, 32, NE], F32)
            ef = kb.sb(st, "ef", [128, 32], F32)
            b_r = Buf()
            rd = [b_lg, b_r, b_rb, b_rc]
            v = lambda t: t[:].rearrange("p n e -> p (n e)")
            v4 = lambda t: t[:].rearrange("p n (g e) -> p (n g) e", e=4)
            g3 = lambda t: t[:].rearrange("p (n g) -> p n g", g=4)
            R = lambda fn: kb.op(dve, fn, reads=rd, writes=[b_r])
            TT = lambda o, a, b_, op: R(lambda: nc.vector.tensor_tensor(out=o, in0=a, in1=b_, op=op))
            RED = lambda o, a, op: R(lambda: nc.vector.tensor_reduce(out=o, in_=a, axis=AX.X, op=op))
            kb.op(act, lambda: nc.scalar.activation(out=v(aff), in_=v(lg), func=AF.Sigmoid), reads=rd, writes=[b_r])
            TT(sel[:], aff[:], bc_mid(rb_bc[:], NS), ALU.add)
            RED(m1[:], v4(sel), ALU.max)
            TT(v4(sel2), v4(sel), bc_last(m1[:], 4), ALU.is_equal)
            R(lambda: nc.vector.scalar_tensor_tensor(out=v(sel2), in0=v(sel2), scalar=-BIG, in1=v(sel),
                                                     op0=ALU.mult, op1=ALU.add))
            RED(m2[:], v4(sel2), ALU.max)
            TT(gs[:], m1[:], m2[:], ALU.add)
            RED(gmax[:], g3(gs), ALU.max)
            TT(g3(gs), g3(gs), bc_last(gmax[:], 4), ALU.is_equal)
            TT(v4(Mk), v4(sel), bc_last(m2[:], 4), ALU.is_ge)
            TT(v4(Mk), v4(Mk), bc_last(gs[:], 4), ALU.mult)
            TT(v(sel2), v(Mk), v(aff), ALU.mult)
            RED(ws[:], sel2[:], ALU.add)
            R(lambda: nc.vector.reciprocal(out=ws[:], in_=ws[:]))
            TT(gts[:], sel2[:], bc_last(ws[:], NE), ALU.mult)
            for n0 in range(0, NS, 32):
                n1 = min(NS, n0 + 32)
                w_ = (n1 - n0) * NE
                mv = Mk[:, n0:n1, :].rearrange("p n e -> p (n e)")
                bk = next_bank()
                mm(bk, bk[0][:, :w_], ones[:], mv, [b_ones, b_r], True, True)
                kb.op(dve, lambda bk=bk, n0=n0, n1=n1, w_=w_: nc.vector.tensor_copy(
                    out=Cn[:, n0:n1, :].rearrange("p n e -> p (n e)"), in_=bk[0][:, :w_]),
                    reads=[bk[1]] + rd, writes=[b_r])
                bk2 = next_bank()
                mm(bk2, bk2[0][:, :w_], rc[:, 0:128], mv, [b_rc, b_r], True, True)
                kb.op(dve, lambda bk2=bk2, n0=n0, n1=n1, w_=w_: nc.vector.tensor_copy(
                    out=Wn[:, n0:n1, :].rearrange("p n e -> p (n e)"), in_=bk2[0][:, :w_]),
                    reads=[bk2[1]] + rd, writes=[b_r])
            R(lambda: nc.vector.memset(off[:, 0, :], 0.0))
            for n in range(1, NS):
                TT(off[:, n, :], off[:, n - 1, :], Cn[:, n - 1, :], ALU.add)
            TT(tot[:], off[:, NS - 1, :], Cn[:, NS - 1, :], ALU.add)
            TT(cmp[:], bc_last(tot[:], 16), bc_mid(rc[:, 128:144], NE), ALU.is_gt)
            RED(ntl[:], cmp[:], ALU.add)
            TT(cmp[:], bc_mid(ntl[:], NE), rc[:, 144:400].rearrange("p (e f) -> p e f", f=16), ALU.mult)
            RED(baseT[:], cmp[:], ALU.add)
            TT(endT[:], baseT[:], ntl[:], ALU.add)
            R(lambda: nc.vector.tensor_scalar(out=base[:], in0=baseT[:], scalar1=512.0, scalar2=None, op0=ALU.mult))
            TT(cmp2[:, :NTILE, :], bc_last(rc[:, 400:400 + NTILE], NE), bc_mid(endT[:], NTILE), ALU.is_ge)
            RED(ef[:, :NTILE], cmp2[:, :NTILE, :], ALU.add)
            R(lambda: nc.vector.tensor_scalar(out=ef[:, :NTILE], in0=ef[:, :NTILE], scalar1=float(NE - 1),
                                              scalar2=128.0, op0=ALU.min, op1=ALU.mult))
            R(lambda: nc.vector.tensor_scalar(out=ef[:, :NTILE], in0=ef[:, :NTILE], scalar1=rc[:, 432:433],
                                              scalar2=None, op0=ALU.add))
            kb.op(dve, lambda: nc.vector.tensor_copy(out=widx[:, :NTILE], in_=ef[:, :NTILE]), reads=rd,
                  writes=[b_route])
            TT(v(posf), v(Wn), v(off), ALU.add)
            TT(posf[:], posf[:], bc_mid(base[:], NS), ALU.add)
            R(lambda: nc.vector.scalar_tensor_tensor(out=v(posf), in0=v(posf), scalar=1.0, in1=v(Mk),
                                                     op0=ALU.add, op1=ALU.mult))
            RED(phi[:], posf[:], ALU.max)
            TT(hiM[:], posf[:], bc_last(phi[:], NE), ALU.is_equal)
            TT(v(tmp), v(hiM), v(gts), ALU.mult)
            kb.op(dve, lambda: nc.vector.tensor_reduce(out=gate2[:, :NS, 1], in_=tmp[:], axis=AX.X, op=ALU.add),
                  reads=rd, writes=[b_route])
            TT(v(hiM), v(Mk), v(hiM), ALU.subtract)
            TT(v(tmp), v(hiM), v(gts), ALU.mult)
            kb.op(dve, lambda: nc.vector.tensor_reduce(out=gate2[:, :NS, 0], in_=tmp[:], axis=AX.X, op=ALU.add),
                  reads=rd, writes=[b_route])
            TT(v(tmp), v(hiM), v(posf), ALU.mult)
            RED(plo[:], tmp[:], ALU.add)
            R(lambda: nc.vector.tensor_scalar(out=pos2f[:, :, 0], in0=plo[:], scalar1=-1.0,
                                              scalar2=float(NSLOT - 1), op0=ALU.add, op1=ALU.min))
            R(lambda: nc.vector.tensor_scalar(out=pos2f[:, :, 1], in0=phi[:], scalar1=-1.0,
                                              scalar2=float(NSLOT - 1), op0=ALU.add, op1=ALU.min))
            R(lambda: nc.vector.tensor_scalar(out=pos2f[:].rearrange("p n k -> p (n k)"),
                                              in0=pos2f[:].rearrange("p n k -> p (n k)"), scalar1=0.0,
                                              scalar2=None, op0=ALU.max))
            kb.op(dve, lambda: nc.vector.tensor_copy(out=pos2i[:, :NS, :].rearrange("p n k -> p (n k)"),
                                                     in_=pos2f[:].rearrange("p n k -> p (n k)")),
                  reads=rd, writes=[b_route])
            if kb.dump:
                gd = kb.dram("gates_l%d" % l, [128, NS, NE], F32)
                kb.dma(sp, gd, gts[:], reads=[b_r])
                pd = kb.dram("pos_l%d" % l, [128, NS, 2], F32)
                kb.dma(sp, pd, pos2f[:], reads=[b_r])
            for n in range(NS):
                for k in range(2):
                    kb.idma(h2s_d[:, :], IOA(ap=pos2i[:, n, k:k + 1], axis=0), h2tok[:, n, :], None,
                            reads=[b_route, b_h2tok[n]])
            kb.barrier()
        return tiles, NTILE

    def moe_experts(l, tiles, NTILE, last):
        wv = wbf_d[l]
        with ExitStack() as st:
            NSL = 6
            wsl = [kb.sb(st, "wsl", [128, 8, D], BF16) for _ in range(NSL)]
            b_wsl = [Buf() for _ in range(NSL)]
            ht = [kb.sb(st, "ht", [128, 4, D], BF16) for _ in range(2)]
            b_ht = [Buf(), Buf()]
            hT = [kb.sb(st, "hT", [128, 8, 512], BF16) for _ in range(2)]
            b_hT = [[Buf() for _ in range(8)] for _ in range(2)]
            he = [kb.sb(st, "he", [128, 8, 512], BF16) for _ in range(2)]
            b_he = [[Buf() for _ in range(8)] for _ in range(2)]
            stmp = [kb.sb(st, "stmp", [128, 512], BF16) for _ in range(2)]
            b_stmp = [Buf(), Buf()]
            ystage = kb.sb(st, "ystage", [128, 4, D], F32)
            b_ys = [Buf() for _ in range(4)]

            def load_w(j, m):
                if j >= NTILE:
                    return
                si = 3 * (j % 2) + m
                kb.idma(wsl[si][:].rearrange("p c f -> p (c f)"), None, wv[m], IOA(ap=widx[:, j:j + 1], axis=0),
                        reads=[b_route], writes=[b_wsl[si]])

            def load_ht(j):
                if j >= NTILE:
                    return
                kb.dma(sp, ht[j % 2][:], h2s_d[j * 512:(j + 1) * 512, :].rearrange("(s p) d -> p s d", p=128),
                       writes=[b_ht[j % 2]])

            def emit_H(j):
                T = 512
                s1, s3 = 3 * (j % 2), 3 * (j % 2) + 1
                htj, bht = ht[j % 2], b_ht[j % 2]
                hb_, bhb = hT[j % 2], b_hT[j % 2]
                for c in range(8):
                    bk = next_bank()
                    for s in range(4):
                        mm(bk, bk[0][:, s * 128:(s + 1) * 128], htj[:, s, c * 128:(c + 1) * 128], identb[:],
                           [bht, b_identb], True, True, tick=(s == 3))
                    if c % 2 == 0:
                        kb.op(act, lambda bk=bk, c=c, hb_=hb_: nc.scalar.copy(out=hb_[:, c, :], in_=bk[0][:, :]),
                              reads=[bk[1]], writes=[bhb[c]])
                    else:
                        kb.op(dve, lambda bk=bk, c=c, hb_=hb_: nc.vector.tensor_copy(out=hb_[:, c, :],
                                                                                   in_=bk[0][:, :]),
                              reads=[bk[1]], writes=[bhb[c]])
                load_ht(j + 2)
                hh, bhh = he[j % 2], b_he[j % 2]
                for fc in range(8):
                    bk1 = next_bank()
                    for k in range(8):
                        mm(bk1, bk1[0][:, :T], wsl[s1][:, k, fc * 128:(fc + 1) * 128], hb_[:, k, :T],
                           [b_wsl[s1], bhb[k]], k == 0, k == 7)
                    bk3 = next_bank()
                    for k in range(8):
                        mm(bk3, bk3[0][:, :T], wsl[s3][:, k, fc * 128:(fc + 1) * 128], hb_[:, k, :T],
                           [b_wsl[s3], bhb[k]], k == 0, k == 7)
                    sm, bsm = stmp[fc % 2], b_stmp[fc % 2]
                    kb.op(act, lambda bk1=bk1, sm=sm, T=T: nc.scalar.activation(out=sm[:, :T], in_=bk1[0][:, :T],
                                                                              func=AF.Silu),
                          reads=[bk1[1]], writes=[bsm])
                    kb.op(dve, lambda fc=fc, bk3=bk3, sm=sm, hh=hh, T=T: nc.vector.tensor_tensor(
                        out=hh[:, fc, :T], in0=bk3[0][:, :T], in1=sm[:, :T], op=ALU.mult),
                        reads=[bk3[1], bsm], writes=[bhh[fc]])

            def emit_Y(j):
                s2 = 3 * (j % 2) + 2
                hh, bhh = he[j % 2], b_he[j % 2]
                for s in range(4):
                    for dh in range(2):
                        bank = next_bank()
                        for fc in range(8):
                            mm(bank, bank[0][:, :], hh[:, fc, s * 128:(s + 1) * 128],
                               wsl[s2][:, fc, dh * 512:(dh + 1) * 512], [bhh[fc], b_wsl[s2]], fc == 0, fc == 7)
                        ya = ystage[:, s, dh * 512:(dh + 1) * 512]
                        if dh == 0:
                            kb.op(act, lambda bank=bank, ya=ya: nc.scalar.copy(out=ya, in_=bank[0][:, :]),
                                  reads=[bank[1]], writes=[b_ys[s]])
                        else:
                            kb.op(dve, lambda bank=bank, ya=ya: nc.vector.tensor_copy(out=ya, in_=bank[0][:, :]),
                                  reads=[bank[1]], writes=[b_ys[s]])
                    r0 = j * 512 + s * 128
                    kb.dma(sp, ys_d[r0:r0 + 128, :], ystage[:, s, :], reads=[b_ys[s]])

            for j in range(2):
                for m in range(3):
                    load_w(j, m)
            load_ht(0)
            load_ht(1)
            emit_H(0)
            load_w(2, 0)
            load_w(2, 1)
            for j in range(NTILE):
                if j + 1 < NTILE:
                    emit_H(j + 1)
                    load_w(j + 3, 0)
                    load_w(j + 3, 1)
                emit_Y(j)
                load_w(j + 2, 2)
            kb.barrier()
        with ExitStack() as st:
            ND = 4
            ga = [[kb.sb(st, "ga", [128, D], F32) for _ in range(2)] for _ in range(ND)]
            b_ga = [[Buf(), Buf()] for _ in range(ND)]
            xs = [kb.sb(st, "xs", [128, D], F32) for _ in range(ND)]
            b_xs = [Buf() for _ in range(ND)]
            sq = kb.sb(st, "sq", [128, D], F32)
            b_sq = Buf()
            ss = kb.sb(st, "ss2", [128, ND], F32)
            b_ss = [Buf() for _ in range(ND)]
            n = 0
            for t in tiles:
                S = t["S"]
                for s, sg in enumerate(t["subs"]):
                    x1, bx1 = xs[n % ND], b_xs[n % ND]
                    g_, bg_ = ga[n % ND], b_ga[n % ND]
                    n += 1
                    r0 = t["ti"] * S.T + s * 128
                    for k in range(2):
                        kb.idma(g_[k][:], None, ys_d[:, :], IOA(ap=pos2i[:, sg, k:k + 1], axis=0),
                                reads=[b_route], writes=[bg_[k]])
                    kb.dma(sp, x1[:], S.xres[r0:r0 + 128, :], reads=[S.b_xres[t["ti"]]], writes=[bx1])
                    kb.op(act, lambda g_=g_, sg=sg: nc.scalar.mul(out=g_[0][:], in_=g_[0][:], mul=gate2[:, sg, 0:1]),
                          reads=[bg_[0], b_route], writes=[bg_[0]])
                    kb.op(dve, lambda g_=g_, sg=sg: nc.vector.scalar_tensor_tensor(
                        out=g_[0][:], in0=g_[1][:], scalar=gate2[:, sg, 1:2], in1=g_[0][:], op0=ALU.mult,
                        op1=ALU.add), reads=[bg_[0], bg_[1], b_route], writes=[bg_[0]])
                    kb.op(dve, lambda g_=g_, S=S: nc.vector.tensor_tensor(out=g_[0][:], in0=g_[0][:],
                                                                         in1=g2[:, S.sidx, :], op=ALU.mult),
                          reads=[bg_[0], b_g2[S.sidx]], writes=[bg_[0]])
                    kb.op(dve, lambda x1=x1, g_=g_: nc.vector.tensor_tensor(out=x1[:], in0=x1[:], in1=g_[0][:],
                                                                         op=ALU.add),
                          reads=[bg_[0], bx1], writes=[bx1])
                    if not last:
                        kb.dma(sp, S.xres[r0:r0 + 128, :], x1[:], reads=[bx1], writes=[S.b_xres[t["ti"]]])
                    else:
                        sc, bsc = ss[:, (n % ND):(n % ND) + 1], b_ss[n % ND]
                        kb.op(act, lambda x1=x1, sc=sc: nc.scalar.activation(
                            out=sq[:], in_=x1[:], func=AF.Square, accum_out=sc),
                            reads=[bx1], writes=[b_sq, bsc])
                        kb.op(dve, lambda sc=sc: nc.vector.tensor_scalar(out=sc, in0=sc, scalar1=1.0 / D,
                                                                        scalar2=EPS, op0=ALU.mult, op1=ALU.add),
                              reads=[bsc], writes=[bsc])
                        kb.op(act, lambda sc=sc: nc.scalar.activation(out=sc, in_=sc, func=AF.Ln),
                              reads=[bsc], writes=[bsc])
                        kb.op(act, lambda sc=sc: nc.scalar.activation(out=sc, in_=sc, func=AF.Exp, scale=-0.5),
                              reads=[bsc], writes=[bsc])
                        kb.op(dve, lambda x1=x1, sc=sc: nc.vector.scalar_tensor_tensor(
                            out=x1[:], in0=x1[:], scalar=sc, in1=gF_bc[:], op0=ALU.mult, op1=ALU.mult),
                            reads=[bx1, bsc, b_gF], writes=[bx1])
                        tk = kb.dma(sp, out_d[r0:r0 + 128, :], x1[:], reads=[bx1])
                        out_toks.append(tk)
            kb.barrier()

    out_toks = []
    GELU = AF.Gelu_apprx_tanh

    def stop(tag):
        return kb.stop_after == tag

    done = False
    for l in range(DEPTH):
        last = l == DEPTH - 1
        with ExitStack() as lst:
            mods = kb.sb(lst, "mods", [128, 2, 3, D], F32)
            b_mods = [[Buf() for _ in range(3)] for _ in range(2)]
            cast_l[0] = l
            mod_phase(l, 0, mods, b_mods, norm_mix_g)
            if l == 0:
                for r0 in range(0, NSLOT, 1024):
                    kb.dma(pool, h2s_d[r0:r0 + 1024, :].rearrange("(p c) d -> p c d", c=8), bc_mid(zrow[:], 8),
                           reads=[b_zrow])
            for d in range(2):
                kb.op(dve, lambda d=d: nc.vector.memset(carry[:, d, :], 0.0), writes=b_carry[d])
            sweep1(SC, l, mods, b_mods, do_four=not last)
            sweep2a_dft(SC, l, need_h=not last, do_dft=not last)
            if stop("ctx_s2a_l%d" % l) or stop("ctx_dft_l%d" % l):
                done = True
                break
            if not last:
                sweep2b(SC, l, mods, b_mods)
                if stop("ctx_mixer_l%d" % l):
                    done = True
                    break
            sweep1(SX, l, mods, b_mods, do_four=True)
            if stop("x_s1_l%d" % l):
                done = True
                break
            sweep2a_dft(SX, l, need_h=True, do_dft=True)
            if stop("x_dft_l%d" % l):
                done = True
                break
            sweep2b(SX, l, mods, b_mods)
            if stop("mixer_l%d" % l):
                done = True
                break
            feed(3 * NE)
            mod_phase(l, 1, mods, b_mods, norm_ffn_g)
            for s in range(2):
                kb.op(dve, lambda s=s: nc.vector.tensor_copy(out=g2[:, s, :], in_=mods[:, s, 2, :]),
                      reads=[b_mods[s][2]], writes=[b_g2[s]])
            tiles, ntile = moe_pre(l, [SX] if last else [SC, SX], mods, b_mods)
            if stop("moe_pre_l%d" % l):
                done = True
                break
        moe_experts(l, tiles, ntile, last)
        if stop("layer_%d" % l):
            done = True
            break

    kb.barrier()
    for tk in out_toks:
        sp.wait(tk)
    kb.es.close()
    return nc


def _consts():
    c = np.arange(128)
    ang = 2.0 * np.pi * np.outer(c, c) / 128.0
    csc = np.concatenate([np.cos(ang), np.sin(ang)], axis=1) / np.sqrt(128.0)

    def tab(n):
        t = np.arange(n, dtype=np.int64)
        m = (np.outer(t, t) % n).astype(np.float64)
        a = 2.0 * np.pi * m / n
        s = 1.0 / np.sqrt(float(n))
        return np.stack([np.cos(a) * s, -np.sin(a) * s]).astype(np.float32).astype(ml_dtypes.bfloat16)

    rcst = np.zeros((128, 448), np.float32)
    rcst[:, 0:128] = (c[:, None] < c[None, :])
    rcst[:, 128:144] = 512.0 * np.arange(16)[None, :]
    e16 = np.arange(16)
    rcst[:, 144:400] = (e16[None, :] < e16[:, None]).astype(np.float32).reshape(1, 256)
    rcst[:, 400:432] = np.arange(32)[None, :]
    rcst[:, 432] = c
    return (np.eye(128, dtype=np.float32), csc.astype(np.float32), tab(L), tab(LC), rcst)


_CACHE = {}


def make_in_maps(inputs):
    if "consts" not in _CACHE:
        _CACHE["consts"] = _consts()
    ident, csc, dftl, dftc, rcst = _CACHE["consts"]
    f = lambda a: np.ascontiguousarray(np.asarray(a, dtype=np.float32))
    shared = {k: f(inputs[k]) for k in (
        "ada_w", "ada_b", "norm_mix_g", "w_in", "conv_w", "conv_b", "lru_w_a", "lru_b_a", "lru_w_x", "lru_b_x",
        "lru_lambda", "w_proj_rnn", "w_proj_fourier", "w_out", "norm_ffn_g", "router_w")}
    for k in ("moe_w1", "moe_w3", "moe_w2"):
        w = f(inputs[k]).reshape(DEPTH, NE, 8, 128, D)
        shared[k] = np.ascontiguousarray(w.transpose(0, 1, 3, 2, 4)).reshape(DEPTH, NE, D, D)
    shared["router_b"] = f(inputs["router_b"]).reshape(1, NE)
    shared["final_norm_g"] = f(inputs["final_norm_g"]).reshape(1, D)
    shared.update(ident=ident, csc=csc, dftl=dftl, dftc=dftc, rconst=rcst)
    x, c, ctx, c_ctx = f(inputs["x"]), f(inputs["c"]), f(inputs["ctx"]), f(inputs["c_ctx"])
    maps = []
    for b in range(8):
        m = dict(shared)
        m["x"] = x[b]
        m["ctx"] = ctx[b]
        m["cc"] = np.ascontiguousarray(np.stack([c[b], c_ctx]))
        maps.append(m)
    return maps


def kernel(**inputs):
    if "nc" not in _CACHE:
        _CACHE["nc"] = build_program()
    nc = _CACHE["nc"]
    maps = make_in_maps(inputs)
    res = run_bass_kernel_spmd(nc, maps, core_ids=list(range(8)))
    return np.stack([np.asarray(r["out"], dtype=np.float32) for r in res.results], axis=0)
```
